# Optimizing a Trainium2 kernel written in Bass

```python
import math
import jax
import jax.numpy as jnp
from jax import lax
import numpy as np


D_MODEL = 1024
BATCH = 8
SEQ = 4096
DEPTH = 4

D_MIX = D_MODEL
D_REC = D_MIX // 2
D_SSM = D_MIX - D_REC
REC_HEADS = 8
REC_HEAD_DIM = D_REC // REC_HEADS
CONV_WIDTH = 4
LRU_C = 8.0
SSM_GROUP = 16
SSM_GROUPS = D_SSM // SSM_GROUP
SSM_STATE = 64
N_EXPERT_GROUPS = 4
EXPERTS_PER_GROUP = 8
N_EXPERTS = N_EXPERT_GROUPS * EXPERTS_PER_GROUP
TOP_K = 2
D_EXPERT = D_MODEL // 2
MOE_BLOCK = 128
ALPHA = (2.0 * DEPTH) ** 0.25
BETA = (8.0 * DEPTH) ** -0.25
LN_EPS = 1e-5
RMS_EPS = 1e-6

kernel_name = 'hybrid_rglru_s5_hmoe'


def layer_norm(x, g, b):
    xf = x.astype(jnp.float32)
    mu = jnp.mean(xf, axis=-1, keepdims=True)
    var = jnp.mean(jnp.square(xf - mu), axis=-1, keepdims=True)
    return ((xf - mu) * lax.rsqrt(var + LN_EPS) * g.astype(jnp.float32) + b.astype(jnp.float32)).astype(x.dtype)


def rms_norm(x, g):
    xf = x.astype(jnp.float32)
    return xf * lax.rsqrt(jnp.mean(jnp.square(xf), axis=-1, keepdims=True) + RMS_EPS) * g.astype(jnp.float32)


def causal_depthwise_conv(u, w, b):
    L = u.shape[1]
    up = jnp.pad(u, ((0, 0), (CONV_WIDTH - 1, 0), (0, 0)))
    out = b
    for k in range(CONV_WIDTH):
        out = out + w[k] * up[:, k:k + L]
    return out


def _linear_combine(c1, c2):
    a1, b1 = c1
    a2, b2 = c2
    return a1 * a2, a2 * b1 + b2


def _complex_linear_combine(c1, c2):
    a1r, a1i, b1r, b1i = c1
    a2r, a2i, b2r, b2i = c2
    ar = a1r * a2r - a1i * a2i
    ai = a1r * a2i + a1i * a2r
    br = a2r * b1r - a2i * b1i + b2r
    bi = a2r * b1i + a2i * b1r + b2i
    return ar, ai, br, bi


def rg_lru(u, wa, ba, wx, bx, lam):
    B, L, _ = u.shape
    uf = u.astype(jnp.float32)
    uh = uf.reshape(B, L, REC_HEADS, REC_HEAD_DIM)
    r = jax.nn.sigmoid(jnp.einsum('blhi,hij->blhj', uh, wa.astype(jnp.float32)).reshape(B, L, D_REC) + ba.astype(jnp.float32))
    i = jax.nn.sigmoid(jnp.einsum('blhi,hij->blhj', uh, wx.astype(jnp.float32)).reshape(B, L, D_REC) + bx.astype(jnp.float32))
    log_a = -LRU_C * r * jax.nn.softplus(-lam.astype(jnp.float32))
    a = jnp.exp(log_a)
    mult = jnp.sqrt(-jnp.expm1(2.0 * log_a))
    bterm = mult * (i * uf)
    _, h = lax.associative_scan(_linear_combine, (a, bterm), axis=1)
    return h


def s5_ssm(u, lam_re, lam_im, log_dt, b_re, b_im, c_re, c_im, d_skip, w_glu, b_glu):
    B, L, _ = u.shape
    uf = u.astype(jnp.float32).reshape(B, L, SSM_GROUPS, SSM_GROUP)
    lre = jnp.minimum(lam_re.astype(jnp.float32), -1e-4)
    lim = lam_im.astype(jnp.float32)
    dt = jnp.exp(log_dt.astype(jnp.float32))[:, None]
    mag = jnp.exp(lre * dt)
    abar_re = mag * jnp.cos(lim * dt)
    abar_im = mag * jnp.sin(lim * dt)
    den = lre * lre + lim * lim
    p_re = abar_re - 1.0
    p_im = abar_im
    coef_re = (p_re * lre + p_im * lim) / den
    coef_im = (p_im * lre - p_re * lim) / den
    br = b_re.astype(jnp.float32)
    bi = b_im.astype(jnp.float32)
    bbar_re = coef_re[..., None] * br - coef_im[..., None] * bi
    bbar_im = coef_re[..., None] * bi + coef_im[..., None] * br
    bu_re = jnp.einsum('blgc,gnc->blgn', uf, bbar_re)
    bu_im = jnp.einsum('blgc,gnc->blgn', uf, bbar_im)
    ar = jnp.broadcast_to(abar_re, bu_re.shape)
    ai = jnp.broadcast_to(abar_im, bu_im.shape)
    _, _, xr, xi = lax.associative_scan(_complex_linear_combine, (ar, ai, bu_re, bu_im), axis=1)
    y = (jnp.einsum('blgn,gcn->blgc', xr, c_re.astype(jnp.float32))
         - jnp.einsum('blgn,gcn->blgc', xi, c_im.astype(jnp.float32))
         + d_skip.astype(jnp.float32) * uf)
    y = jax.nn.gelu(y.reshape(B, L, D_SSM))
    z = y @ w_glu.astype(jnp.float32) + b_glu.astype(jnp.float32)
    return z[..., :D_SSM] * jax.nn.sigmoid(z[..., D_SSM:])


def hybrid_mixer(x, w_in, conv_w, conv_b, lru_wa, lru_ba, lru_wx, lru_bx, lru_lambda,
                 ssm_lambda_re, ssm_lambda_im, ssm_log_dt, ssm_b_re, ssm_b_im, ssm_c_re, ssm_c_im,
                 ssm_d, w_glu, b_glu, g_rec, g_ssm, w_out):
    proj = x @ w_in
    gate_br = proj[..., :D_REC]
    rec_br = proj[..., D_REC:2 * D_REC]
    ssm_br = proj[..., 2 * D_REC:]
    rec = causal_depthwise_conv(rec_br, conv_w, conv_b)
    h = rg_lru(rec, lru_wa, lru_ba, lru_wx, lru_bx, lru_lambda)
    y_rec = jax.nn.gelu(gate_br.astype(jnp.float32)) * h
    y_ssm = s5_ssm(ssm_br, ssm_lambda_re, ssm_lambda_im, ssm_log_dt, ssm_b_re, ssm_b_im,
                   ssm_c_re, ssm_c_im, ssm_d, w_glu, b_glu)
    y = jnp.concatenate([rms_norm(y_rec, g_rec), rms_norm(y_ssm, g_ssm)], axis=-1).astype(x.dtype)
    return y @ w_out


def hierarchical_moe(x, router_wg, router_bg, router_we, router_be, exp_w_gate, exp_w_up, exp_w_down):
    B, L, D = x.shape
    T = B * L
    xt = x.reshape(T, D)
    g_prob = jax.nn.softmax((xt @ router_wg).astype(jnp.float32) + router_bg.astype(jnp.float32), axis=-1)
    g_top, g_idx = lax.top_k(g_prob, 1)
    e_logits = ((xt @ router_we).astype(jnp.float32) + router_be.astype(jnp.float32)).reshape(T, N_EXPERT_GROUPS, EXPERTS_PER_GROUP)
    e_in = jnp.take_along_axis(e_logits, g_idx[:, :, None], axis=1)[:, 0]
    e_prob = jax.nn.softmax(e_in, axis=-1)
    e_top, e_idx = lax.top_k(e_prob, TOP_K)
    gates = g_top * e_top / jnp.sum(e_top, axis=-1, keepdims=True)
    expert_id = (g_idx * EXPERTS_PER_GROUP + e_idx).reshape(-1)
    token_id = jnp.repeat(jnp.arange(T, dtype=jnp.int32), TOP_K)
    gate_flat = gates.reshape(-1)
    order = jnp.argsort(expert_id)
    sorted_e = expert_id[order]
    counts = jnp.bincount(expert_id, length=N_EXPERTS)
    starts = jnp.cumsum(counts) - counts
    padded = (counts + MOE_BLOCK - 1) // MOE_BLOCK * MOE_BLOCK
    pad_ends = jnp.cumsum(padded)
    pad_starts = pad_ends - padded
    j = jnp.arange(T * TOP_K, dtype=jnp.int32)
    dest = pad_starts[sorted_e] + j - starts[sorted_e]
    n_blocks = -(-(T * TOP_K) // MOE_BLOCK) + N_EXPERTS
    P = n_blocks * MOE_BLOCK
    buf_tok = jnp.full((P,), T, dtype=jnp.int32).at[dest].set(token_id[order])
    buf_gate = jnp.zeros((P,), jnp.float32).at[dest].set(gate_flat[order])
    block_e = jnp.minimum(jnp.searchsorted(pad_ends, jnp.arange(n_blocks, dtype=jnp.int32) * MOE_BLOCK, side='right'), N_EXPERTS - 1)
    x_pad = jnp.concatenate([xt, jnp.zeros((1, D), xt.dtype)], axis=0)
    xb = x_pad[buf_tok].reshape(n_blocks, MOE_BLOCK, D)

    def expert_block(args):
        xblk, e = args
        hdn = jax.nn.silu(xblk @ exp_w_gate[e]) * (xblk @ exp_w_up[e])
        return hdn @ exp_w_down[e]

    yb = lax.map(expert_block, (xb, block_e)).reshape(P, D)
    y = jax.ops.segment_sum(yb * buf_gate[:, None].astype(yb.dtype), buf_tok, num_segments=T + 1)[:T]
    return y.reshape(B, L, D)


def setup_inputs(seed: int = 0) -> dict:
    key = jax.random.key(seed)
    ks = jax.random.split(key, 40)
    Lc = DEPTH
    f32 = jnp.float32

    def nrm(k, shape, scale):
        return jax.random.normal(k, shape, f32) * scale

    x = nrm(ks[0], (BATCH, SEQ, D_MODEL), 1.0)
    w_in = nrm(ks[1], (Lc, D_MODEL, 2 * D_REC + D_SSM), D_MODEL ** -0.5)
    conv_w = nrm(ks[2], (Lc, CONV_WIDTH, D_REC), CONV_WIDTH ** -0.5)
    conv_b = nrm(ks[3], (Lc, D_REC), 0.01)
    lru_wa = nrm(ks[4], (Lc, REC_HEADS, REC_HEAD_DIM, REC_HEAD_DIM), REC_HEAD_DIM ** -0.5)
    lru_ba = nrm(ks[5], (Lc, D_REC), 0.01)
    lru_wx = nrm(ks[6], (Lc, REC_HEADS, REC_HEAD_DIM, REC_HEAD_DIM), REC_HEAD_DIM ** -0.5)
    lru_bx = nrm(ks[7], (Lc, D_REC), 0.01)
    a0 = jax.random.uniform(ks[8], (Lc, D_REC), f32, minval=0.9, maxval=0.999)
    lru_lambda = jnp.log(a0) - jnp.log1p(-a0)
    n_idx = jnp.arange(SSM_STATE, dtype=f32)
    ssm_lambda_re = -0.5 + nrm(ks[9], (Lc, SSM_GROUPS, SSM_STATE), 0.01)
    ssm_lambda_im = jnp.pi * n_idx + nrm(ks[10], (Lc, SSM_GROUPS, SSM_STATE), 0.01)
    ssm_log_dt = jax.random.uniform(ks[11], (Lc, SSM_GROUPS), f32, minval=math.log(1e-3), maxval=math.log(1e-1))
    ssm_b_re = nrm(ks[12], (Lc, SSM_GROUPS, SSM_STATE, SSM_GROUP), (2.0 * SSM_GROUP) ** -0.5)
    ssm_b_im = nrm(ks[13], (Lc, SSM_GROUPS, SSM_STATE, SSM_GROUP), (2.0 * SSM_GROUP) ** -0.5)
    ssm_c_re = nrm(ks[14], (Lc, SSM_GROUPS, SSM_GROUP, SSM_STATE), SSM_STATE ** -0.5)
    ssm_c_im = nrm(ks[15], (Lc, SSM_GROUPS, SSM_GROUP, SSM_STATE), SSM_STATE ** -0.5)
    ssm_d = nrm(ks[16], (Lc, SSM_GROUPS, SSM_GROUP), 1.0)
    w_glu = nrm(ks[17], (Lc, D_SSM, 2 * D_SSM), D_SSM ** -0.5)
    b_glu = nrm(ks[18], (Lc, 2 * D_SSM), 0.01)
    g_rec = 1.0 + nrm(ks[19], (Lc, D_REC), 0.01)
    g_ssm = 1.0 + nrm(ks[20], (Lc, D_SSM), 0.01)
    w_out = nrm(ks[21], (Lc, D_MIX, D_MODEL), (D_MIX ** -0.5) * BETA)
    ln1_g = 1.0 + nrm(ks[22], (Lc, D_MODEL), 0.01)
    ln1_b = nrm(ks[23], (Lc, D_MODEL), 0.01)
    router_wg = nrm(ks[24], (Lc, D_MODEL, N_EXPERT_GROUPS), D_MODEL ** -0.5)
    router_bg = nrm(ks[25], (Lc, N_EXPERT_GROUPS), 0.01)
    router_we = nrm(ks[26], (Lc, D_MODEL, N_EXPERTS), D_MODEL ** -0.5)
    router_be = nrm(ks[27], (Lc, N_EXPERTS), 0.01)
    exp_w_gate = nrm(ks[28], (Lc, N_EXPERTS, D_MODEL, D_EXPERT), D_MODEL ** -0.5)
    exp_w_up = nrm(ks[29], (Lc, N_EXPERTS, D_MODEL, D_EXPERT), D_MODEL ** -0.5)
    exp_w_down = nrm(ks[30], (Lc, N_EXPERTS, D_EXPERT, D_MODEL), (D_EXPERT ** -0.5) * BETA)
    ln2_g = 1.0 + nrm(ks[31], (Lc, D_MODEL), 0.01)
    ln2_b = nrm(ks[32], (Lc, D_MODEL), 0.01)
    return {'x': x, 'w_in': w_in, 'conv_w': conv_w, 'conv_b': conv_b,
            'lru_wa': lru_wa, 'lru_ba': lru_ba, 'lru_wx': lru_wx, 'lru_bx': lru_bx, 'lru_lambda': lru_lambda,
            'ssm_lambda_re': ssm_lambda_re, 'ssm_lambda_im': ssm_lambda_im, 'ssm_log_dt': ssm_log_dt,
            'ssm_b_re': ssm_b_re, 'ssm_b_im': ssm_b_im, 'ssm_c_re': ssm_c_re, 'ssm_c_im': ssm_c_im,
            'ssm_d': ssm_d, 'w_glu': w_glu, 'b_glu': b_glu, 'g_rec': g_rec, 'g_ssm': g_ssm,
            'w_out': w_out, 'ln1_g': ln1_g, 'ln1_b': ln1_b,
            'router_wg': router_wg, 'router_bg': router_bg, 'router_we': router_we, 'router_be': router_be,
            'exp_w_gate': exp_w_gate, 'exp_w_up': exp_w_up, 'exp_w_down': exp_w_down,
            'ln2_g': ln2_g, 'ln2_b': ln2_b}


def reference(x, w_in, conv_w, conv_b, lru_wa, lru_ba, lru_wx, lru_bx, lru_lambda,
              ssm_lambda_re, ssm_lambda_im, ssm_log_dt, ssm_b_re, ssm_b_im, ssm_c_re, ssm_c_im,
              ssm_d, w_glu, b_glu, g_rec, g_ssm, w_out, ln1_g, ln1_b,
              router_wg, router_bg, router_we, router_be, exp_w_gate, exp_w_up, exp_w_down,
              ln2_g, ln2_b):
    for l in range(DEPTH):
        mix = hybrid_mixer(x, w_in[l], conv_w[l], conv_b[l], lru_wa[l], lru_ba[l], lru_wx[l], lru_bx[l],
                           lru_lambda[l], ssm_lambda_re[l], ssm_lambda_im[l], ssm_log_dt[l],
                           ssm_b_re[l], ssm_b_im[l], ssm_c_re[l], ssm_c_im[l], ssm_d[l],
                           w_glu[l], b_glu[l], g_rec[l], g_ssm[l], w_out[l])
        x = layer_norm(ALPHA * x + mix, ln1_g[l], ln1_b[l])
        ff = hierarchical_moe(x, router_wg[l], router_bg[l], router_we[l], router_be[l],
                              exp_w_gate[l], exp_w_up[l], exp_w_down[l])
        x = layer_norm(ALPHA * x + ff, ln2_g[l], ln2_b[l])
    return x
```

```python
import math
from contextlib import ExitStack

import numpy as np
import concourse.bass as bass
import concourse.mybir as mybir
from concourse.bass_utils import run_bass_kernel_spmd

F32 = mybir.dt.float32
BF16 = mybir.dt.bfloat16
I32 = mybir.dt.int32
AF = mybir.ActivationFunctionType
ALU = mybir.AluOpType
AX = mybir.AxisListType

ENGS = ("pe", "act", "dve", "pool", "sp")


class _Rec:
    def __init__(self):
        self.call = None

    def __getattr__(self, name):
        def f(*a, **k):
            self.call = (name, a, k)
            return self
        return f


def _record(fn):
    r = _Rec()
    fn(r)
    assert r.call is not None
    return r.call


class Sched:
    def __init__(self, nc):
        self.nc = nc
        self.ops = {e: [] for e in ENGS}
        self.seq = {e: 0 for e in ENGS}
        self.waited = {e: {} for e in ENGS}
        self.last_w = {}
        self.readers = {}
        self.dcount = {}
        self.sems = {}
        self.dsem_names = []

    def _deps(self, eng, reads, writes):
        need = {}

        def add(dep):
            if dep is None:
                return
            k, v = dep
            if k.startswith("d:"):
                v = self.dcount[k[2:]]
            if need.get(k, 0) < v:
                need[k] = v

        for b in reads:
            add(self.last_w.get(b))
        for b in writes:
            add(self.last_w.get(b))
            for r in self.readers.get(b, ()):
                add(r)
        out = []
        w = self.waited[eng]
        for k, v in need.items():
            if eng == "pe" and k == "pe":
                continue
            if w.get(k, 0) < v:
                w[k] = v
                out.append((k, v))
        return out

    def _commit(self, token, reads, writes):
        for b in reads:
            self.readers.setdefault(b, []).append(token)
        for b in writes:
            self.last_w[b] = token
            self.readers[b] = []

    enabled = True

    def op(self, eng, fn, reads=(), writes=()):
        if not self.enabled:
            return
        self.commit_op(eng, _record(fn), reads, writes)

    def commit_op(self, eng, rec, reads=(), writes=()):
        if not self.enabled:
            return
        writes = list(writes) + [k for k in reads if k.startswith("ps")]
        reads = [k for k in reads if not k.startswith("ps")]
        waits = self._deps(eng, reads, writes)
        self.seq[eng] += 1
        token = (eng, self.seq[eng])
        self._commit(token, reads, writes)
        self.ops[eng].append(("op", rec, waits, None))

    def dma(self, eng, fn, dsem, reads=(), writes=()):
        if not self.enabled:
            return
        self.commit_dma(eng, _record(fn), dsem, reads, writes)

    def commit_dma(self, eng, rec, dsem, reads=(), writes=()):
        if not self.enabled:
            return
        waits = self._deps(eng, reads, writes)
        if dsem not in self.dcount:
            self.dcount[dsem] = 0
            self.dsem_names.append(dsem)
        self.dcount[dsem] += 16
        token = ("d:" + dsem, self.dcount[dsem])
        self._commit(token, reads, writes)
        self.ops[eng].append(("dma", rec, waits, dsem))

    def barrier(self):
        for e in ENGS:
            waits = []
            w = self.waited[e]
            for o in ENGS:
                if o != e and self.seq[o] > w.get(o, 0):
                    w[o] = self.seq[o]
                    waits.append((o, self.seq[o]))
            for d, c in self.dcount.items():
                k = "d:" + d
                if c > w.get(k, 0):
                    w[k] = c
                    waits.append((k, c))
            self.ops[e].append(("wait", None, waits, None))

    def emit(self):
        nc = self.nc
        with ExitStack() as es:
            for e in ENGS:
                self.sems[e] = es.enter_context(nc.semaphore("s_" + e))
            for d in self.dsem_names:
                self.sems["d:" + d] = es.enter_context(nc.semaphore("d_" + d))
            block = es.enter_context(nc.Block())
            engmap = {"pe": block.tensor, "act": block.scalar, "dve": block.vector,
                      "pool": block.gpsimd, "sp": block.sync}
            for e in ENGS:
                def body(eng, lst=self.ops[e], e=e):
                    regs = {}
                    for kind, fn, waits, dsem in lst:
                        for k, v in waits:
                            eng.wait_ge(self.sems[k], v)
                        if fn is not None and isinstance(fn[2].get("bounds_check"), int):
                            bc = fn[2]["bounds_check"]
                            if bc not in regs:
                                regs[bc] = eng.to_reg(bc)
                            fn = (fn[0], fn[1], dict(fn[2], bounds_check=regs[bc]))
                        if kind == "op":
                            getattr(eng, fn[0])(*fn[1], **fn[2]).then_inc(self.sems[e], 1)
                        elif kind == "dma":
                            getattr(eng, fn[0])(*fn[1], **fn[2]).then_inc(self.sems["d:" + dsem], 16)
                engmap[e](body)


class _Stop(Exception):
    pass


class Cfg:
    stop = None

    def __init__(self, **kw):
        self.L = 4
        self.D = 1024
        self.DR = 512
        self.DS = 512
        self.DE = 512
        self.T = 4096
        self.TT = 256
        self.TS = 128
        self.C = 384
        self.NG = 4
        self.EPG = 8
        for k, v in kw.items():
            setattr(self, k, v)
        c = self
        c.E = c.NG * c.EPG
        c.KD, c.KR, c.KS, c.KE = c.D // 128, c.DR // 128, c.DS // 128, c.DE // 128
        c.GP = c.DS // 32
        c.DP = 2 * c.DR + c.DS
        c.KP = c.DP // 128
        c.JT = c.TT // 128
        c.NTT = c.T // c.TT
        c.NJ = c.T // 128
        c.NST = c.TT // c.TS
        c.CB = c.C // 128
        c.DW = min(c.D, 512)
        c.NDH = c.D // c.DW
        c.NR = c.NG + c.E
        c.ALPHA = (2.0 * c.L) ** 0.25
        o = 0
        c.o = {}
        for name, n in (("convw", c.KR * 4), ("convb", c.KR), ("ba", c.KR), ("bx", c.KR),
                        ("lam", c.KR), ("grec", c.KR), ("bglu", 2 * c.KS), ("gssm", c.KS),
                        ("ssmd", c.KS), ("lre", c.GP), ("lim", c.GP), ("ldt", c.GP)):
            c.o[name] = (o, n)
            o += n
        c.NPP = o
        c.NROW = 4 * c.D + c.NR


def build(c):
    nc = bass.Bass("TRN2", target_bir_lowering=False)
    L, D, T, E, C = c.L, c.D, c.T, c.E, c.C

    def din(name, shape, dt=F32):
        return nc.dram_tensor(name, list(shape), dt, kind="ExternalInput").ap()

    x_in = din("x", [T, D])
    w_in = din("w_in", [L, D, c.DP])
    w_glu = din("w_glu", [L, c.DS, 2 * c.DS])
    w_out = din("w_out", [L, D, D])
    lruA = din("lruA", [L, c.KR, 128, 128])
    lruX = din("lruX", [L, c.KR, 128, 128])
    Bre = din("Bre", [L, 128, c.GP, 128])
    Bim = din("Bim", [L, 128, c.GP, 128])
    Cre = din("Cre", [L, 128, c.GP, 32])
    Cim = din("Cim", [L, 128, c.GP, 32])
    pp_d = din("pp", [L, 128, c.NPP])
    rows_d = din("rows", [L, 128, c.NROW])
    wr_d = din("wr", [L, D, c.NR])
    wg_d = din("wg", [L, E, D, c.DE])
    wu_d = din("wu", [L, E, D, c.DE])
    wd_d = din("wd", [L, E, c.DE, D])
    ident_d = din("ident", [128, 128])
    ustr_d = din("ustrict", [128, 128])
    ebase_d = din("ebase", [128, E + 1])
    y_out = nc.dram_tensor("y", [T, D], F32, kind="ExternalOutput").ap()
    xs = nc.dram_tensor("xs", [T, D], F32, kind="Internal").ap()
    x1s = nc.dram_tensor("x1s", [T, D], F32, kind="Internal").ap()
    xg = nc.dram_tensor("xg", [E * C + 128, D], BF16, kind="Internal").ap()
    yg = nc.dram_tensor("yg", [E * C + 128, D], F32, kind="Internal").ap()

    S = Sched(nc)
    op, dma = S.op, S.dma
    BIG = float(E * C + 64)
    TT, TS, JT = c.TT, c.TS, c.JT
    KD, KR, KS, KE, GP = c.KD, c.KR, c.KS, c.KE, c.GP

    with ExitStack() as es:
        def sb(name, shape, dt=F32):
            return es.enter_context(nc.sbuf_tensor("t_" + name, list(shape), dt))

        def pst(name, shape, dt=F32):
            return es.enter_context(nc.psum_tensor("p_" + name, list(shape), dt))

        ident = sb("ident", [128, 128])
        identb = sb("identb", [128, 128], BF16)
        ustr = sb("ustr", [128, 128], BF16)
        onesb = sb("onesb", [128, 128], BF16)
        ebase = sb("ebase", [128, E + 1])
        stage = [sb("stage%d" % i, [128, 2048]) for i in range(2)]
        SLOTS = sb("SLOTS", [128, c.NJ, 2], I32)
        GATES = sb("GATES", [128, c.NJ, 2])
        ps = [pst("ps%d" % i, [128, 512]) for i in range(7)]
        psT = pst("psT", [128, 1024], BF16)

        stage_ctr = [0]
        stage_pool = [list(stage), "stage"]

        def load_cast(dst_ap, src_ap, ncols, dst_key, shape3=None, eng="pool", defer=None):
            tiles, pfx = stage_pool
            i = stage_ctr[0] % len(tiles)
            stage_ctr[0] += 1
            st = tiles[i]
            sv = st[:, 0:ncols]
            if shape3 is not None:
                sv = sv.rearrange("p (a b) -> p a b", a=shape3[0])
            key = "%s%d" % (pfx, i)
            dma("sp", lambda e: e.dma_start(out=sv, in_=src_ap), key, writes=[key])

            def do_cast():
                if eng == "act":
                    op("act", lambda e: e.copy(out=dst_ap, in_=sv), reads=[key], writes=[dst_key])
                else:
                    op(eng, lambda e: e.tensor_copy(out=dst_ap, in_=sv), reads=[key], writes=[dst_key])

            if defer is not None:
                defer.append(do_cast)
            else:
                do_cast()

        dma("sp", lambda e: e.dma_start(out=ident[:], in_=ident_d), "cst", writes=["ident"])
        dma("sp", lambda e: e.dma_start(out=ebase[:], in_=ebase_d), "cst", writes=["ebase"])
        op("dve", lambda e: e.tensor_copy(out=identb[:], in_=ident[:]), reads=["ident"], writes=["identb"])
        load_cast(ustr[:], ustr_d, 128, "ustr")
        op("dve", lambda e: e.memset(onesb[:], 1.0), writes=["onesb"])
        with nc.sbuf_tensor("t_zt", [128, D], BF16) as zt, nc.sbuf_tensor("t_zf32", [128, D], F32) as zf32:
            op("dve", lambda e: e.memset(zt[:], 0.0), writes=["zt"])
            op("dve", lambda e: e.memset(zf32[:], 0.0), writes=["zf32"])
            dma("sp", lambda e: e.dma_start(out=yg[E * C:E * C + 128, :], in_=zf32[:]), "zf", reads=["zf32"])
            for r0 in range(0, E * C + 128, 128):
                dma("sp", lambda e, r0=r0: e.dma_start(out=xg[r0:r0 + 128, :], in_=zt[:]), "zf",
                    reads=["zt"])
        S.barrier()

        cur_tt = [0]

        def ck(name):
            if c.stop == name or c.stop == "%s@%d" % (name, cur_tt[0]):
                S.enabled = False

        try:
          for l in range(L):
              xcur = x_in if l == 0 else xs
              xnext = y_out if l == L - 1 else xs
              with ExitStack() as em:
                  def sm(name, shape, dt=F32):
                      return em.enter_context(nc.sbuf_tensor("m%d_%s" % (l, name), list(shape), dt))

                  wib = sm("wib", [128, KD, c.DP], BF16)
                  wglub = sm("wglub", [128, KS, 2 * c.DS], BF16)
                  woutb = sm("woutb", [128, KD, D], BF16)
                  lruAb = sm("lruAb", [128, KR, 128], BF16)
                  lruXb = sm("lruXb", [128, KR, 128], BF16)
                  Breb = sm("Breb", [128, GP, 128], BF16)
                  Bimb = sm("Bimb", [128, GP, 128], BF16)
                  Cpre = sm("Cpre", [128, GP, 128], BF16)
                  Cpim = sm("Cpim", [128, GP, 128], BF16)
                  wrf = sm("wrf", [128, KD, c.NR])
                  pp = sm("pp", [128, c.NPP])
                  rows = sm("rows", [128, 2 * D + c.NR])
                  CS = sm("CS", [128, GP, TS])
                  SN = sm("SN", [128, GP, TS])
                  sm_ = {}
                  for nm in ("sp1", "c1", "c2", "lre", "dt", "ldt", "rho", "th", "q", "qf", "thk", "are", "aim",
                             "pre", "den", "t0", "t1", "cre", "cim", "ncim"):
                      sm_[nm] = sm("s_" + nm, [128, max(GP, KR)])
                  qi = sm("s_qi", [128, GP], I32)
                  hcar = sm("hcar", [128, KR])
                  xrc = sm("xrc", [128, GP])
                  xic = sm("xic", [128, GP])
                  cw = [sm("cw%d" % i, [128, 4]) for i in range(4)]
                  Mcum = sm("Mcum", [128, E])
                  Mcumb = sm("Mcumb", [128, E], BF16)
                  xt = [sm("xt%d" % j, [128, D]) for j in range(JT)]
                  xb = sm("xb", [128, D], BF16)
                  xfm = sm("xfm", [128, KD, TT], BF16)
                  gateg = sm("gateg", [128, KR, TT])
                  recin = sm("recin", [128, KR, TT + 3])
                  uf = sm("uf", [128, KS, TT])
                  ub = sm("ub", [128, KS, TT], BF16)
                  rw = {nm: sm("rw_" + nm, [128, TT]) for nm in ("rc", "r", "i", "a", "mu", "bt", "h", "yr")}
                  rcb = sm("rcb", [128, TT], BF16)
                  sw = {nm: sm("sw_" + nm, [128, 4 * TS]) for nm in
                        ("ta", "tb", "br", "bi", "wr0", "wi0", "wr1", "wi1", "ma", "mb")}
                  assert GP * 32 <= 4 * TS
                  cview = lambda t: t[:, 0:GP * 32].rearrange("p (g m) -> p g m", g=GP)
                  Cre_f, Cim_f = cview(sw["ta"]), cview(sw["tb"])
                  ctmp = [cview(sw["ma"]), cview(sw["mb"])]
                  xrb = [sm("xrb%d" % i, [128, 4 * TS], BF16) for i in range(2)]
                  xib = [sm("xib%d" % i, [128, 4 * TS], BF16) for i in range(2)]
                  ysq = sm("ysq", [128, KD, TT], BF16)
                  ybf = sm("ybf", [128, KD, TT], BF16)
                  gy = sm("gy", [128, KS, TT], BF16)
                  ypre = sm("ypre", [128, KS, TT])
                  sgl = sm("sgl", [128, TT])
                  yss = sm("yss", [128, TT])
                  zz = sm("zz", [128, D])
                  x1 = sm("x1", [128, D])
                  x1T = sm("x1T", [128, KD, 128])
                  bst = sm("bst", [128, 2 * c.NDH, 6])
                  mv = sm("mv", [128, 2])
                  rt = {nm: sm("rt_" + nm, [128, 1]) for nm in
                        ("ssr", "sss", "rsr", "rss", "sd", "rstd", "gmax", "ngmax", "gsum", "gtop", "d12", "s1", "s2",
                         "sl1", "sl2", "v1", "v2", "g1", "g2")}
                  RB = []
                  for j_ in range(JT):
                      d_ = {}
                      for nm, shp, dt_ in (("lg", [128, c.NR], F32), ("exg", [128, c.NG], F32), ("gm", [128, c.NG], F32),
                                           ("etmp", [128, E], F32), ("ein", [128, c.EPG], F32), ("top8", [128, 8], F32),
                                           ("sel1", [128, c.EPG], F32), ("sel2", [128, c.EPG], F32), ("oh1", [128, E], F32),
                                           ("oh2", [128, E], F32), ("Mt", [128, E], F32), ("Mb", [128, E], BF16),
                                           ("valid", [128, E], F32), ("val", [128, E], F32), ("slf", [128, 2], F32),
                                           ("x1b", [128, D], BF16)):
                          d_[nm] = sm("rb%d_%s" % (j_, nm), shp, dt_)
                      d_["rt"] = {nm: sm("rb%d_rt_%s" % (j_, nm), [128, 1]) for nm in
                                  ("gmax", "ngmax", "gsum", "gtop", "d12", "s1", "s2", "v1", "v2", "g1", "g2")}
                      RB.append(d_)
                  CHAIN_KEYS = set(["lg", "exg", "gm", "etmp", "ein", "top8", "sel1", "sel2", "oh1", "oh2", "Mt", "Mb",
                                    "valid", "val", "slf", "x1b"] + ["rt_" + n for n in
                                   ("gmax", "ngmax", "gsum", "gtop", "d12", "s1", "s2", "v1", "v2", "g1", "g2")])

                  def P(name, i=None):
                      o, n = c.o[name]
                      if i is None:
                          return pp[:, o:o + n]
                      return pp[:, o + i:o + i + 1]

                  stage_pool[0] = list(stage)
                  stage_pool[1] = "stage"
                  stage_ctr[0] = 0
                  dma("sp", lambda e: e.dma_start(out=pp[:], in_=pp_d[l]), "lw", writes=["pp"])
                  dma("sp", lambda e: e.dma_start(out=rows[:, 0:2 * D], in_=rows_d[l][:, 0:2 * D]), "lw", writes=["rows"])
                  dma("sp", lambda e: e.dma_start(out=rows[:, 2 * D:2 * D + c.NR], in_=rows_d[l][:, 4 * D:4 * D + c.NR]),
                      "lw", writes=["rows"])
                  dma("sp", lambda e: e.dma_start(out=wrf[:], in_=wr_d[l].rearrange("(k p) n -> p k n", p=128)), "lw",
                      writes=["wrf"])
                  dma("sp", lambda e: e.dma_start(out=Cre_f, in_=Cre[l]), "lw", writes=["sw_ta"])
                  dma("sp", lambda e: e.dma_start(out=Cim_f, in_=Cim[l]), "lw", writes=["sw_tb"])
                  win_v = w_in[l].rearrange("(k p) f -> p k f", p=128)
                  fstep = 2048 // 512 * 512 if c.DP >= 512 else c.DP
                  for kc in range(KD):
                      for f0 in range(0, c.DP, 2048):
                          f1 = min(c.DP, f0 + 2048)
                          load_cast(wib[:, kc, f0:f1], win_v[:, kc, f0:f1], f1 - f0, "wib")
                  wgl_v = w_glu[l].rearrange("(k p) f -> p k f", p=128)
                  for kc in range(KS):
                      load_cast(wglub[:, kc, :], wgl_v[:, kc, :], 2 * c.DS, "wglub")
                  wo_v = w_out[l].rearrange("(k p) f -> p k f", p=128)
                  for kc in range(KD):
                      load_cast(woutb[:, kc, :], wo_v[:, kc, :], D, "woutb")
                  for kr in range(KR):
                      load_cast(lruAb[:, kr, :], lruA[l, kr], 128, "lruAb")
                      load_cast(lruXb[:, kr, :], lruX[l, kr], 128, "lruXb")
                  for g0 in range(0, GP, 16):
                      g1 = min(GP, g0 + 16)
                      load_cast(Breb[:, g0:g1, :], Bre[l][:, g0:g1, :], (g1 - g0) * 128, "Breb", shape3=(g1 - g0, 128))
                      load_cast(Bimb[:, g0:g1, :], Bim[l][:, g0:g1, :], (g1 - g0) * 128, "Bimb", shape3=(g1 - g0, 128))
                  op("pool", lambda e: e.memset(Cpre[:], 0.0), writes=["Cpre"])
                  op("pool", lambda e: e.memset(Cpim[:], 0.0), writes=["Cpim"])

                  g = lambda nm, n=GP: sm_[nm][:, 0:n]
                  op("act", lambda e: e.activation(out=g("sp1", KR), in_=P("lam"), func=AF.Exp, scale=-1.0),
                     reads=["pp"], writes=["s_sp1"])
                  op("act", lambda e: e.activation(out=g("sp1", KR), in_=g("sp1", KR), func=AF.Ln, bias=1.0),
                     reads=["s_sp1"], writes=["s_sp1"])
                  op("dve", lambda e: e.tensor_scalar(out=g("c1", KR), in0=g("sp1", KR), scalar1=-8.0, scalar2=None,
                                                      op0=ALU.mult), reads=["s_sp1"], writes=["s_c1"])
                  op("dve", lambda e: e.tensor_scalar(out=g("c2", KR), in0=g("sp1", KR), scalar1=-16.0, scalar2=None,
                                                      op0=ALU.mult), reads=["s_sp1"], writes=["s_c2"])
                  op("dve", lambda e: e.tensor_scalar(out=g("lre"), in0=P("lre"), scalar1=-1e-4, scalar2=None,
                                                      op0=ALU.min), reads=["pp"], writes=["s_lre"])
                  op("act", lambda e: e.activation(out=g("dt"), in_=P("ldt"), func=AF.Exp), reads=["pp"], writes=["s_dt"])
                  op("dve", lambda e: e.tensor_tensor(out=g("ldt"), in0=g("lre"), in1=g("dt"), op=ALU.mult),
                     reads=["s_lre", "s_dt"], writes=["s_ldt"])
                  op("act", lambda e: e.activation(out=g("rho"), in_=g("ldt"), func=AF.Exp), reads=["s_ldt"],
                     writes=["s_rho"])
                  op("dve", lambda e: e.tensor_tensor(out=g("th"), in0=P("lim"), in1=g("dt"), op=ALU.mult),
                     reads=["pp", "s_dt"], writes=["s_th"])
                  op("dve", lambda e: e.tensor_scalar(out=g("q"), in0=g("th"), scalar1=1.0 / (2 * math.pi), scalar2=None,
                                                      op0=ALU.mult), reads=["s_th"], writes=["s_q"])
                  op("dve", lambda e: e.tensor_copy(out=qi[:], in_=g("q")), reads=["s_q"], writes=["s_qi"])
                  op("dve", lambda e: e.tensor_copy(out=g("qf"), in_=qi[:]), reads=["s_qi"], writes=["s_qf"])
                  op("dve", lambda e: e.scalar_tensor_tensor(out=g("thk"), in0=g("qf"), scalar=-2 * math.pi, in1=g("th"),
                                                             op0=ALU.mult, op1=ALU.add),
                     reads=["s_qf", "s_th"], writes=["s_thk"])
                  def wrap(ap, key, scr, skey):
                      op("dve", lambda e: e.tensor_scalar(out=scr, in0=ap, scalar1=math.pi, scalar2=None, op0=ALU.is_gt),
                         reads=[key], writes=[skey])
                      op("dve", lambda e: e.scalar_tensor_tensor(out=ap, in0=scr, scalar=-2 * math.pi, in1=ap,
                                                                 op0=ALU.mult, op1=ALU.add), reads=[skey, key], writes=[key])
                      op("dve", lambda e: e.tensor_scalar(out=scr, in0=ap, scalar1=-math.pi, scalar2=None, op0=ALU.is_lt),
                         reads=[key], writes=[skey])
                      op("dve", lambda e: e.scalar_tensor_tensor(out=ap, in0=scr, scalar=2 * math.pi, in1=ap,
                                                                 op0=ALU.mult, op1=ALU.add), reads=[skey, key], writes=[key])

                  wrap(g("thk"), "s_thk", g("t0"), "s_t0")
                  op("dve", lambda e: e.tensor_copy(out=CS[:, :, 0:1], in_=sm_["thk"][:, 0:GP].unsqueeze(2)),
                     reads=["s_thk"], writes=["CS"])
                  w = 1
                  while w < TS:
                      op("dve", lambda e, w=w: e.tensor_tensor(
                          out=CS[:, :, w:2 * w], in0=CS[:, :, 0:w],
                          in1=sm_["thk"][:, 0:GP].unsqueeze(2).to_broadcast([128, GP, w]), op=ALU.add),
                         reads=["CS", "s_thk"], writes=["CS"])
                      wrap(CS[:, :, w:2 * w], "CS", SN[:, :, w:2 * w], "SN")
                      op("dve", lambda e: e.tensor_tensor(out=g("thk"), in0=g("thk"), in1=g("thk"), op=ALU.add),
                         reads=["s_thk"], writes=["s_thk"])
                      wrap(g("thk"), "s_thk", g("t0"), "s_t0")
                      w *= 2
                  op("act", lambda e: e.activation(out=SN[:], in_=CS[:], func=AF.Sin), reads=["CS"], writes=["SN"])
                  op("dve", lambda e: e.scalar_tensor_tensor(out=CS[:], in0=CS[:], scalar=-1.0, in1=CS[:], op0=ALU.mult,
                                                             op1=ALU.max), reads=["CS", "SN"], writes=["CS"])
                  op("act", lambda e: e.activation(out=CS[:], in_=CS[:], func=AF.Sin, scale=-1.0, bias=math.pi / 2),
                     reads=["CS"], writes=["CS"])
                  cs0 = CS[:, :, 0:1].rearrange("p g o -> p (g o)")
                  sn0 = SN[:, :, 0:1].rearrange("p g o -> p (g o)")
                  op("dve", lambda e: e.tensor_tensor(out=g("are"), in0=g("rho"), in1=cs0, op=ALU.mult),
                     reads=["s_rho", "CS"], writes=["s_are"])
                  op("dve", lambda e: e.tensor_tensor(out=g("aim"), in0=g("rho"), in1=sn0, op=ALU.mult),
                     reads=["s_rho", "SN"], writes=["s_aim"])
                  op("dve", lambda e: e.tensor_scalar(out=g("pre"), in0=g("are"), scalar1=-1.0, scalar2=None, op0=ALU.add),
                     reads=["s_are"], writes=["s_pre"])
                  op("dve", lambda e: e.tensor_tensor(out=g("den"), in0=g("lre"), in1=g("lre"), op=ALU.mult),
                     reads=["s_lre"], writes=["s_den"])
                  op("dve", lambda e: e.tensor_tensor(out=g("t0"), in0=P("lim"), in1=P("lim"), op=ALU.mult),
                     reads=["pp"], writes=["s_t0"])
                  op("dve", lambda e: e.tensor_tensor(out=g("den"), in0=g("den"), in1=g("t0"), op=ALU.add),
                     reads=["s_den", "s_t0"], writes=["s_den"])
                  op("dve", lambda e: e.reciprocal(out=g("den"), in_=g("den")), reads=["s_den"], writes=["s_den"])
                  op("dve", lambda e: e.tensor_tensor(out=g("t0"), in0=g("pre"), in1=g("lre"), op=ALU.mult),
                     reads=["s_pre", "s_lre"], writes=["s_t0"])
                  op("dve", lambda e: e.tensor_tensor(out=g("t1"), in0=g("aim"), in1=P("lim"), op=ALU.mult),
                     reads=["s_aim", "pp"], writes=["s_t1"])
                  op("dve", lambda e: e.tensor_tensor(out=g("t0"), in0=g("t0"), in1=g("t1"), op=ALU.add),
                     reads=["s_t0", "s_t1"], writes=["s_t0"])
                  op("dve", lambda e: e.tensor_tensor(out=g("cre"), in0=g("t0"), in1=g("den"), op=ALU.mult),
                     reads=["s_t0", "s_den"], writes=["s_cre"])
                  op("dve", lambda e: e.tensor_tensor(out=g("t0"), in0=g("aim"), in1=g("lre"), op=ALU.mult),
                     reads=["s_aim", "s_lre"], writes=["s_t0"])
                  op("dve", lambda e: e.tensor_tensor(out=g("t1"), in0=g("pre"), in1=P("lim"), op=ALU.mult),
                     reads=["s_pre", "pp"], writes=["s_t1"])
                  op("dve", lambda e: e.tensor_tensor(out=g("t0"), in0=g("t0"), in1=g("t1"), op=ALU.subtract),
                     reads=["s_t0", "s_t1"], writes=["s_t0"])
                  op("dve", lambda e: e.tensor_tensor(out=g("cim"), in0=g("t0"), in1=g("den"), op=ALU.mult),
                     reads=["s_t0", "s_den"], writes=["s_cim"])
                  creb = sm_["cre"][:, 0:GP].unsqueeze(2).to_broadcast([128, GP, 32])
                  cimb = sm_["cim"][:, 0:GP].unsqueeze(2).to_broadcast([128, GP, 32])
                  op("dve", lambda e: e.tensor_tensor(out=ctmp[0], in0=Cre_f, in1=creb, op=ALU.mult),
                     reads=["sw_ta", "s_cre"], writes=["sw_ma"])
                  op("dve", lambda e: e.tensor_tensor(out=ctmp[1], in0=Cim_f, in1=cimb, op=ALU.mult),
                     reads=["sw_tb", "s_cim"], writes=["sw_mb"])
                  w4 = lambda ap, q, lo, hi: ap.rearrange("p (k q) m -> p k q m", q=4)[:, :, q, lo:hi]
                  for q4 in range(4):
                      op("dve", lambda e, q4=q4: e.tensor_tensor(
                          out=w4(Cpre[:], q4, q4 * 32, (q4 + 1) * 32), in0=w4(ctmp[0], q4, 0, 32),
                          in1=w4(ctmp[1], q4, 0, 32), op=ALU.subtract), reads=["sw_ma", "sw_mb"], writes=["Cpre"])
                  op("dve", lambda e: e.tensor_tensor(out=ctmp[0], in0=Cre_f, in1=cimb, op=ALU.mult),
                     reads=["sw_ta", "s_cim", "Cpre"], writes=["sw_ma"])
                  op("dve", lambda e: e.tensor_tensor(out=ctmp[1], in0=Cim_f, in1=creb, op=ALU.mult),
                     reads=["sw_tb", "s_cre", "Cpre"], writes=["sw_mb"])
                  for q4 in range(4):
                      op("dve", lambda e, q4=q4: e.scalar_tensor_tensor(
                          out=w4(Cpim[:], q4, q4 * 32, (q4 + 1) * 32), in0=w4(ctmp[0], q4, 0, 32), scalar=-1.0,
                          in1=w4(ctmp[1], q4, 0, 32), op0=ALU.mult, op1=ALU.subtract),
                         reads=["sw_ma", "sw_mb"], writes=["Cpim"])
                  op("dve", lambda e: e.memset(hcar[:], 0.0), writes=["hcar"])
                  op("dve", lambda e: e.memset(xrc[:], 0.0), writes=["xrc"])
                  op("dve", lambda e: e.memset(xic[:], 0.0), writes=["xic"])
                  op("dve", lambda e: e.memset(recin[:], 0.0), writes=["recin"])
                  op("dve", lambda e: e.memset(Mcum[:], 0.0), writes=["Mcum"])
                  op("dve", lambda e: e.memset(Mcumb[:], 0.0), writes=["Mcumb"])

                  ck("setup")
                  for tt in range(c.NTT):
                      t0 = tt * TT
                      cur_tt[0] = tt
                      if tt == 1:
                          ck("T1")
                      for j in range(JT):
                          dma("sp", lambda e, j=j: e.dma_start(out=xt[j][:], in_=xcur[t0 + j * 128:t0 + (j + 1) * 128, :]),
                              "xt%d" % j, writes=["xt%d" % j])
                          op("pool", lambda e, j=j: e.tensor_copy(out=xb[:], in_=xt[j][:]), reads=["xt%d" % j],
                             writes=["xb"])
                          for k0 in range(0, KD, 4):
                              kn = min(4, KD - k0)
                              for kk in range(kn):
                                  op("pe", lambda e, kk=kk, k0=k0: e.transpose(
                                      out=psT[:, kk * 128:(kk + 1) * 128],
                                      in_=xb[:, (k0 + kk) * 128:(k0 + kk + 1) * 128], identity=identb[:]),
                                     reads=["xb", "identb"], writes=["psT"])
                              op("act", lambda e, k0=k0, kn=kn, j=j: e.copy(
                                  out=xfm[:, k0:k0 + kn, j * 128:(j + 1) * 128],
                                  in_=psT[:, 0:kn * 128].rearrange("p (k t) -> p k t", k=kn)),
                                 reads=["psT"], writes=["xfm"])
                      ck("M2")
                      for fc in range(c.KP):
                          ck("M3f%d" % fc)
                          pb = ps[fc % 2]
                          pk = "ps%d" % (fc % 2)
                          for kc in range(KD):
                              op("pe", lambda e, fc=fc, kc=kc, pb=pb: e.matmul(
                                  pb[:, 0:TT], lhsT=wib[:, kc, fc * 128:(fc + 1) * 128], rhs=xfm[:, kc, :],
                                  start=(kc == 0), stop=(kc == KD - 1)), reads=["wib", "xfm"], writes=[pk])
                          if fc < KR:
                              op("act", lambda e, fc=fc, pb=pb: e.activation(out=gateg[:, fc, :], in_=pb[:, 0:TT],
                                                                              func=AF.Gelu), reads=[pk], writes=["gateg"])
                          elif fc < 2 * KR:
                              op("act", lambda e, fc=fc, pb=pb: e.copy(out=recin[:, fc - KR, 3:3 + TT], in_=pb[:, 0:TT]),
                                 reads=[pk], writes=["recin"])
                          else:
                              op("act", lambda e, fc=fc, pb=pb: e.copy(out=uf[:, fc - 2 * KR, :], in_=pb[:, 0:TT]),
                                 reads=[pk], writes=["uf"])
                              op("dve", lambda e, fc=fc, pb=pb: e.tensor_copy(out=ub[:, fc - 2 * KR, :], in_=pb[:, 0:TT]),
                                 reads=[pk], writes=["ub"])
                      ck("M3")
                      def rec_a(kr):
                          rc, r_, i_, a_, mu, bt, h_, yr = (rw[n] for n in ("rc", "r", "i", "a", "mu", "bt", "h", "yr"))
                          cwo = c.o["convw"][0] + kr * 4
                          op("dve", lambda e, kr=kr, cwo=cwo: e.tensor_scalar(
                              out=rc[:], in0=recin[:, kr, 0:TT], scalar1=pp[:, cwo:cwo + 1], scalar2=P("convb", kr),
                              op0=ALU.mult, op1=ALU.add), reads=["recin", "pp"], writes=["rw_rc"])
                          for k in range(1, 4):
                              op("dve", lambda e, kr=kr, k=k, cwo=cwo: e.scalar_tensor_tensor(
                                  out=rc[:], in0=recin[:, kr, k:k + TT], scalar=pp[:, cwo + k:cwo + k + 1], in1=rc[:],
                                  op0=ALU.mult, op1=ALU.add), reads=["recin", "pp", "rw_rc"], writes=["rw_rc"])
                          op("pool", lambda e: e.tensor_copy(out=rcb[:], in_=rc[:]), reads=["rw_rc"], writes=["rcb"])
                          op("pe", lambda e, kr=kr: e.matmul(ps[3][:, 0:TT], lhsT=lruAb[:, kr, :], rhs=rcb[:],
                                                             start=True, stop=True), reads=["lruAb", "rcb"], writes=["ps3"])
                          op("pe", lambda e, kr=kr: e.matmul(ps[3][:, TT:2 * TT], lhsT=lruXb[:, kr, :], rhs=rcb[:],
                                                             start=True, stop=True), reads=["lruXb", "rcb"], writes=["ps3"])
                          op("act", lambda e, kr=kr: e.activation(out=r_[:], in_=ps[3][:, 0:TT], func=AF.Sigmoid,
                                                                  bias=P("ba", kr)), reads=["ps3", "pp"], writes=["rw_r"])
                          op("act", lambda e, kr=kr: e.activation(out=i_[:], in_=ps[3][:, TT:2 * TT], func=AF.Sigmoid,
                                                                  bias=P("bx", kr)), reads=["ps3", "pp"], writes=["rw_i"])
                          op("act", lambda e, kr=kr: e.activation(out=a_[:], in_=r_[:], func=AF.Exp,
                                                                  scale=sm_["c1"][:, kr:kr + 1]),
                             reads=["rw_r", "s_c1"], writes=["rw_a"])
                          op("act", lambda e, kr=kr: e.activation(out=mu[:], in_=r_[:], func=AF.Exp,
                                                                  scale=sm_["c2"][:, kr:kr + 1]),
                             reads=["rw_r", "s_c2"], writes=["rw_mu"])
                          op("act", lambda e: e.activation(out=mu[:], in_=mu[:], func=AF.Sqrt, scale=-1.0, bias=1.0),
                             reads=["rw_mu"], writes=["rw_mu"])
                          op("pool", lambda e: e.tensor_tensor(out=bt[:], in0=i_[:], in1=rc[:], op=ALU.mult),
                             reads=["rw_i", "rw_rc"], writes=["rw_bt"])
                          op("pool", lambda e: e.tensor_tensor(out=bt[:], in0=bt[:], in1=mu[:], op=ALU.mult),
                             reads=["rw_bt", "rw_mu"], writes=["rw_bt"])
                      def rec_b(kr):
                          rc, r_, i_, a_, mu, bt, h_, yr = (rw[n] for n in ("rc", "r", "i", "a", "mu", "bt", "h", "yr"))
                          op("dve", lambda e, kr=kr: e.tensor_tensor_scan(
                              out=h_[:], data0=a_[:], data1=bt[:], initial=hcar[:, kr:kr + 1], op0=ALU.mult, op1=ALU.add),
                             reads=["rw_a", "rw_bt", "hcar"], writes=["rw_h"])
                          op("dve", lambda e, kr=kr: e.tensor_copy(out=hcar[:, kr:kr + 1], in_=h_[:, TT - 1:TT]),
                             reads=["rw_h"], writes=["hcar"])
                          op("dve", lambda e, kr=kr: e.tensor_tensor(out=yr[:], in0=gateg[:, kr, :], in1=h_[:],
                                                                     op=ALU.mult), reads=["gateg", "rw_h"], writes=["rw_yr"])
                          op("act", lambda e, kr=kr: e.activation(out=ysq[:, kr, :], in_=yr[:], func=AF.Square),
                             reads=["rw_yr"], writes=["ysq"])
                          op("pool", lambda e, kr=kr: e.tensor_scalar(out=ybf[:, kr, :], in0=yr[:], scalar1=P("grec", kr),
                                                                      scalar2=0.0, op0=ALU.mult, op1=ALU.add),
                             reads=["rw_yr", "pp"], writes=["ybf"])
                          op("pool", lambda e, kr=kr: e.tensor_copy(out=recin[:, kr, 0:3], in_=recin[:, kr, TT:TT + 3]),
                             reads=["recin", "rw_rc"], writes=["recin"])
                      ck("M4")
                      W4 = 4 * TS
                      f4 = lambda ap: ap.rearrange("p g t -> p (g t)")
                      g4 = lambda ap: ap.rearrange("p (g t) -> p g t", g=4)
                      if True:
                          s5_late = []

                          def s5_carry(ks, par, WR, WI, kwr, kwi, gsl):
                              wrl = g4(WR[:])[:, :, TS - 1:TS].rearrange("p g o -> p (g o)")
                              wil = g4(WI[:])[:, :, TS - 1:TS].rearrange("p g o -> p (g o)")
                              csl = CS[:, gsl, TS - 1:TS].rearrange("p g o -> p (g o)")
                              snl = SN[:, gsl, TS - 1:TS].rearrange("p g o -> p (g o)")
                              op("pool", lambda e: e.tensor_tensor(out=cw[0][:], in0=wrl, in1=csl, op=ALU.mult),
                                 reads=[kwr, "CS"], writes=["cw0"])
                              op("pool", lambda e: e.tensor_tensor(out=cw[1][:], in0=wil, in1=snl, op=ALU.mult),
                                 reads=[kwi, "SN"], writes=["cw1"])
                              op("pool", lambda e: e.tensor_tensor(out=cw[2][:], in0=wil, in1=csl, op=ALU.mult),
                                 reads=[kwi, "CS"], writes=["cw2"])
                              op("pool", lambda e: e.tensor_tensor(out=cw[3][:], in0=wrl, in1=snl, op=ALU.mult),
                                 reads=[kwr, "SN"], writes=["cw3"])
                              op("pool", lambda e: e.tensor_tensor(out=xrc[:, gsl], in0=cw[0][:], in1=cw[1][:],
                                                                  op=ALU.subtract), reads=["cw0", "cw1"], writes=["xrc"])
                              op("pool", lambda e: e.tensor_tensor(out=xic[:, gsl], in0=cw[2][:], in1=cw[3][:],
                                                                  op=ALU.add), reads=["cw2", "cw3"], writes=["xic"])

                          def s5_iter(st, ks, par):
                              pvr, kvr = (ps[4], "ps4") if par == 0 else (ps[0], "ps0")
                              pvi, kvi = (ps[5], "ps5") if par == 0 else (ps[1], "ps1")
                              py, kpy = (ps[6], "ps6") if par == 0 else (ps[2], "ps2")
                              tsl = slice(st * TS, (st + 1) * TS)
                              for q in range(4):
                                  gp = ks * 4 + q
                                  op("pe", lambda e, gp=gp, q=q, pvr=pvr: e.matmul(
                                      pvr[:, q * TS:(q + 1) * TS], lhsT=Breb[:, gp, :], rhs=ub[:, ks, tsl],
                                      start=True, stop=True), reads=["Breb", "ub"], writes=[kvr])
                              for q in range(4):
                                  gp = ks * 4 + q
                                  op("pe", lambda e, gp=gp, q=q, pvi=pvi: e.matmul(
                                      pvi[:, q * TS:(q + 1) * TS], lhsT=Bimb[:, gp, :], rhs=ub[:, ks, tsl],
                                      start=True, stop=True), reads=["Bimb", "ub"], writes=[kvi])
                              CS4 = f4(CS[:, ks * 4:(ks + 1) * 4, :])
                              SN4 = f4(SN[:, ks * 4:(ks + 1) * 4, :])
                              TA, TB, BR, BI = sw["ta"], sw["tb"], sw["br"], sw["bi"]
                              WR, WI = sw["wr%d" % par], sw["wi%d" % par]
                              kwr, kwi = "sw_wr%d" % par, "sw_wi%d" % par
                              op("dve", lambda e: e.tensor_tensor(out=TA[:], in0=pvr[:, 0:W4], in1=CS4, op=ALU.mult),
                                 reads=[kvr, "CS"], writes=["sw_ta"])
                              op("dve", lambda e: e.tensor_tensor(out=TB[:], in0=pvi[:, 0:W4], in1=SN4, op=ALU.mult),
                                 reads=[kvi, "SN"], writes=["sw_tb"])
                              op("dve", lambda e: e.tensor_tensor(out=BR[:], in0=TA[:], in1=TB[:], op=ALU.add),
                                 reads=["sw_ta", "sw_tb"], writes=["sw_br"])
                              op("dve", lambda e: e.tensor_tensor(out=TA[:], in0=pvi[:, 0:W4], in1=CS4, op=ALU.mult),
                                 reads=[kvi, "CS", "sw_br"], writes=["sw_ta"])
                              op("dve", lambda e: e.tensor_tensor(out=TB[:], in0=pvr[:, 0:W4], in1=SN4, op=ALU.mult),
                                 reads=[kvr, "SN", "sw_br"], writes=["sw_tb"])
                              op("dve", lambda e: e.tensor_tensor(out=BI[:], in0=TA[:], in1=TB[:], op=ALU.subtract),
                                 reads=["sw_ta", "sw_tb"], writes=["sw_bi"])
                              for q in range(4):
                                  gp = ks * 4 + q
                                  qs = slice(q * TS, (q + 1) * TS)
                                  rho_b = sm_["rho"][:, gp:gp + 1].to_broadcast([128, TS])
                                  op("dve", lambda e, gp=gp, qs=qs, rho_b=rho_b: e.tensor_tensor_scan(
                                      out=WR[:, qs], data0=rho_b, data1=BR[:, qs], initial=xrc[:, gp:gp + 1],
                                      op0=ALU.mult, op1=ALU.add), reads=["sw_br", "xrc", "s_rho"], writes=[kwr])
                                  op("dve", lambda e, gp=gp, qs=qs, rho_b=rho_b: e.tensor_tensor_scan(
                                      out=WI[:, qs], data0=rho_b, data1=BI[:, qs], initial=xic[:, gp:gp + 1],
                                      op0=ALU.mult, op1=ALU.add), reads=["sw_bi", "xic", "s_rho"], writes=[kwi])
                              prev_late = list(s5_late)
                              del s5_late[:]
                              for fn_ in prev_late:
                                  fn_()
                              gsl = slice(ks * 4, (ks + 1) * 4)
                              if KS > 1:
                                  s5_late.append(lambda: s5_carry(ks, par, WR, WI, kwr, kwi, gsl))
                              else:
                                  s5_carry(ks, par, WR, WI, kwr, kwi, gsl)
                              MA, MB = sw["ma"], sw["mb"]
                              XR, XI = xrb[par], xib[par]
                              op("pool", lambda e: e.tensor_tensor(out=MA[:], in0=WR[:], in1=CS4, op=ALU.mult),
                                 reads=[kwr, "CS"], writes=["sw_ma"])
                              op("pool", lambda e: e.tensor_tensor(out=MB[:], in0=WI[:], in1=SN4, op=ALU.mult),
                                 reads=[kwi, "SN"], writes=["sw_mb"])
                              op("pool", lambda e: e.tensor_tensor(out=XR[:], in0=MA[:], in1=MB[:], op=ALU.subtract),
                                 reads=["sw_ma", "sw_mb"], writes=["xrb%d" % par])
                              op("pool", lambda e: e.tensor_tensor(out=MA[:], in0=WI[:], in1=CS4, op=ALU.mult),
                                 reads=[kwi, "CS", "xrb%d" % par], writes=["sw_ma"])
                              op("pool", lambda e: e.tensor_tensor(out=MB[:], in0=WR[:], in1=SN4, op=ALU.mult),
                                 reads=[kwr, "SN", "xrb%d" % par], writes=["sw_mb"])
                              op("pool", lambda e: e.tensor_tensor(out=XI[:], in0=MA[:], in1=MB[:], op=ALU.add),
                                 reads=["sw_ma", "sw_mb"], writes=["xib%d" % par])
                              for q in range(4):
                                  gp = ks * 4 + q
                                  qs = slice(q * TS, (q + 1) * TS)
                                  op("pe", lambda e, gp=gp, q=q, qs=qs, py=py: e.matmul(
                                      py[:, 0:TS], lhsT=Cpre[:, gp, :], rhs=XR[:, qs], start=(q == 0), stop=False),
                                     reads=["Cpre", "xrb%d" % par], writes=[kpy])
                                  op("pe", lambda e, gp=gp, q=q, qs=qs, py=py: e.matmul(
                                      py[:, 0:TS], lhsT=Cpim[:, gp, :], rhs=XI[:, qs], start=False, stop=(q == 3)),
                                     reads=["Cpim", "xib%d" % par], writes=[kpy])
                              s5_late.append(lambda: op("dve", lambda e, ks=ks, py=py: e.scalar_tensor_tensor(
                                  out=ypre[:, ks, tsl], in0=uf[:, ks, tsl], scalar=P("ssmd", ks), in1=py[:, 0:TS],
                                  op0=ALU.mult, op1=ALU.add), reads=["uf", "pp", kpy], writes=["ypre"]))
                      its = [(st_, ks_) for st_ in range(c.NST) for ks_ in range(KS)]
                      per = max(1, -(-len(its) // KR))
                      nrec = 0
                      pend_b = None
                      for ii, (st_, ks_) in enumerate(its):
                          s5_iter(st_, ks_, ii % 2)
                          if pend_b is not None:
                              rec_b(pend_b)
                              pend_b = None
                          elif nrec < KR:
                              rec_a(nrec)
                              pend_b = nrec
                              nrec += 1
                      for fn_ in s5_late:
                          fn_()
                      del s5_late[:]
                      if pend_b is not None:
                          rec_b(pend_b)
                      while nrec < KR:
                          rec_a(nrec)
                          rec_b(nrec)
                          nrec += 1
                      for ks in range(KS):
                          op("act", lambda e, ks=ks: e.activation(out=gy[:, ks, :], in_=ypre[:, ks, :], func=AF.Gelu),
                             reads=["ypre"], writes=["gy"])
                      ck("M5")
                      for fo in range(KS):
                          for half, pb, pk in ((0, ps[2], "ps2"), (1, ps[3], "ps3")):
                              col = half * c.DS + fo * 128
                              for ks in range(KS):
                                  op("pe", lambda e, ks=ks, col=col, pb=pb: e.matmul(
                                      pb[:, 0:TT], lhsT=wglub[:, ks, col:col + 128], rhs=gy[:, ks, :],
                                      start=(ks == 0), stop=(ks == KS - 1)), reads=["wglub", "gy"], writes=[pk])
                          op("act", lambda e, fo=fo: e.activation(out=sgl[:], in_=ps[3][:, 0:TT], func=AF.Sigmoid,
                                                                  bias=P("bglu", KS + fo)),
                             reads=["ps3", "pp"], writes=["sgl"])
                          op("dve", lambda e, fo=fo: e.scalar_tensor_tensor(
                              out=yss[:], in0=ps[2][:, 0:TT], scalar=P("bglu", fo), in1=sgl[:], op0=ALU.add, op1=ALU.mult),
                             reads=["ps2", "pp", "sgl"], writes=["yss"])
                          op("act", lambda e, fo=fo: e.activation(out=ysq[:, KR + fo, :], in_=yss[:], func=AF.Square),
                             reads=["yss"], writes=["ysq"])
                          op("pool", lambda e, fo=fo: e.tensor_scalar(out=ybf[:, KR + fo, :], in0=yss[:],
                                                                      scalar1=P("gssm", fo), scalar2=0.0, op0=ALU.mult, op1=ALU.add),
                             reads=["yss", "pp"], writes=["ybf"])
                      ck("GLU")
                      chains = []
                      for j in range(JT):
                          jg = tt * JT + j
                          tsl = slice(j * 128, (j + 1) * 128)
                          for kc in range(KD):
                              half = 0 if kc < KR else 1
                              first = kc in (0, KR)
                              last = kc in (KR - 1, KD - 1)
                              op("pe", lambda e, kc=kc, half=half, first=first, last=last: e.matmul(
                                  ps[4][:, half:half + 1], lhsT=ysq[:, kc, tsl], rhs=onesb[:, 0:1], start=first, stop=last),
                                 reads=["ysq", "onesb"], writes=["ps4"])
                          op("act", lambda e: e.activation(out=rt["ssr"][:], in_=ps[4][:, 0:1], func=AF.Sqrt,
                                                           scale=1.0 / c.DR, bias=1e-6), reads=["ps4"], writes=["rt_ssr"])
                          op("act", lambda e: e.activation(out=rt["sss"][:], in_=ps[4][:, 1:2], func=AF.Sqrt,
                                                           scale=1.0 / c.DS, bias=1e-6), reads=["ps4"], writes=["rt_sss"])
                          op("dve", lambda e: e.reciprocal(out=rt["rsr"][:], in_=rt["ssr"][:]), reads=["rt_ssr"],
                             writes=["rt_rsr"])
                          op("dve", lambda e: e.reciprocal(out=rt["rss"][:], in_=rt["sss"][:]), reads=["rt_sss"],
                             writes=["rt_rss"])
                          op("act", lambda e, j=j: e.mul(out=xt[j][:], in_=xt[j][:], mul=c.ALPHA), reads=["xt%d" % j],
                             writes=["xt%d" % j])
                          for dh in range(c.NDH):
                              dsl = slice(dh * c.DW, (dh + 1) * c.DW)
                              for half, pb, pk in ((0, ps[0], "ps0"), (1, ps[1], "ps1")):
                                  kcs = range(0, KR) if half == 0 else range(KR, KD)
                                  for kc in kcs:
                                      op("pe", lambda e, kc=kc, pb=pb, kcs=kcs: e.matmul(
                                          pb[:, 0:c.DW], lhsT=ybf[:, kc, tsl], rhs=woutb[:, kc, dsl],
                                          start=(kc == kcs[0]), stop=(kc == kcs[-1])), reads=["ybf", "woutb"], writes=[pk])
                              op("dve", lambda e, dsl=dsl, j=j: e.scalar_tensor_tensor(
                                  out=zz[:, dsl], in0=ps[0][:, 0:c.DW], scalar=rt["rsr"][:], in1=xt[j][:, dsl],
                                  op0=ALU.mult, op1=ALU.add), reads=["ps0", "rt_rsr", "xt%d" % j], writes=["zz"])
                              op("dve", lambda e, dsl=dsl: e.scalar_tensor_tensor(
                                  out=zz[:, dsl], in0=ps[1][:, 0:c.DW], scalar=rt["rss"][:], in1=zz[:, dsl],
                                  op0=ALU.mult, op1=ALU.add), reads=["ps1", "rt_rss", "zz"], writes=["zz"])
                          layer_norm(c, op, zz, x1, bst, mv, rt, rows[:, 0:D], rows[:, D:2 * D], "zz", "x1", "rows")
                          dma("sp", lambda e, jg=jg: e.dma_start(out=x1s[jg * 128:(jg + 1) * 128, :], in_=x1[:]), "x1st",
                              reads=["x1"])
                          op("act", lambda e, j=j: e.copy(out=RB[j]["x1b"][:], in_=x1[:]), reads=["x1"], writes=["x1b#%d" % j])
                          ck("LN1")
                          for k0 in range(0, KD, 4):
                              kn = min(4, KD - k0)
                              for kk in range(kn):
                                  op("pe", lambda e, kk=kk, k0=k0: e.transpose(
                                      out=ps[5][:, kk * 128:(kk + 1) * 128], in_=x1[:, (k0 + kk) * 128:(k0 + kk + 1) * 128],
                                      identity=ident[:]), reads=["x1", "ident"], writes=["ps5"])
                              op("act", lambda e, k0=k0, kn=kn: e.copy(
                                  out=x1T[:, k0:k0 + kn, :], in_=ps[5][:, 0:kn * 128].rearrange("p (k t) -> p k t", k=kn)),
                                 reads=["ps5"], writes=["x1T"])
                          for kc in range(KD):
                              op("pe", lambda e, kc=kc: e.matmul(ps[6][:, 0:c.NR], lhsT=x1T[:, kc, :], rhs=wrf[:, kc, :],
                                                                 start=(kc == 0), stop=(kc == KD - 1)),
                                 reads=["x1T", "wrf"], writes=["ps6"])
                          ck("RT0")
                          NG, EPG = c.NG, c.EPG
                          rb = RB[j]
                          lg, exg, gm, etmp, ein, top8, sel1, sel2, oh1, oh2, Mt, Mb, valid, val, slf, x1b = (
                              rb[n] for n in ("lg", "exg", "gm", "etmp", "ein", "top8", "sel1", "sel2", "oh1", "oh2", "Mt",
                                              "Mb", "valid", "val", "slf", "x1b"))
                          rtj = rb["rt"]
                          ppos, kpos = (ps[6], "ps6") if j % 2 == 0 else (ps[4], "ps4")
                          chain = []
                          chains.append(chain)
                          km = lambda ks_, j=j: [(k + "#%d" % j) if k in CHAIN_KEYS else k for k in ks_]

                          def cop(eng, fn, reads=(), writes=(), chain=chain, km=km):
                              chain.append(("op", eng, _record(fn), None, km(reads), km(writes)))

                          def cdma(eng, fn, dsem, reads=(), writes=(), chain=chain, km=km):
                              chain.append(("dma", eng, _record(fn), dsem, km(reads), km(writes)))

                          op("dve", lambda e: e.tensor_tensor(out=lg[:], in0=ps[6][:, 0:c.NR], in1=rows[:, 2 * D:2 * D + c.NR],
                                                              op=ALU.add), reads=["ps6", "rows"], writes=["lg#%d" % j])
                          cop("dve", lambda e: e.tensor_reduce(out=rtj["gmax"][:], in_=lg[:, 0:NG], axis=AX.X, op=ALU.max),
                             reads=["lg"], writes=["rt_gmax"])
                          cop("dve", lambda e: e.tensor_scalar(out=rtj["ngmax"][:], in0=rtj["gmax"][:], scalar1=-1.0,
                                                              scalar2=None, op0=ALU.mult), reads=["rt_gmax"],
                             writes=["rt_ngmax"])
                          cop("act", lambda e: e.activation(out=exg[:], in_=lg[:, 0:NG], func=AF.Exp, bias=rtj["ngmax"][:],
                                                           accum_out=rtj["gsum"][:]), reads=["lg", "rt_ngmax"],
                             writes=["exg", "rt_gsum"])
                          cop("dve", lambda e: e.reciprocal(out=rtj["gtop"][:], in_=rtj["gsum"][:]), reads=["rt_gsum"],
                             writes=["rt_gtop"])
                          cop("dve", lambda e: e.tensor_scalar(out=gm[:], in0=lg[:, 0:NG], scalar1=rtj["gmax"][:],
                                                              scalar2=None, op0=ALU.is_equal), reads=["lg", "rt_gmax"],
                             writes=["gm"])
                          e3 = lambda ap: ap.rearrange("p (g e) -> p g e", g=NG)
                          gmb = gm[:].unsqueeze(2).to_broadcast([128, NG, EPG])
                          cop("dve", lambda e: e.tensor_tensor(out=e3(etmp[:]), in0=e3(lg[:, NG:NG + c.E]), in1=gmb,
                                                              op=ALU.mult), reads=["lg", "gm"], writes=["etmp"])
                          cop("dve", lambda e: e.tensor_reduce(out=ein[:], in_=etmp[:].rearrange("p (g e) -> p e g", g=NG),
                                                              axis=AX.X, op=ALU.add), reads=["etmp"], writes=["ein"])
                          cop("dve", lambda e: e.max(out=top8[:], in_=ein[:]), reads=["ein"], writes=["top8"])
                          cop("dve", lambda e: e.tensor_scalar(out=sel1[:], in0=ein[:], scalar1=top8[:, 0:1], scalar2=None,
                                                              op0=ALU.is_equal), reads=["ein", "top8"], writes=["sel1"])
                          cop("dve", lambda e: e.tensor_scalar(out=sel2[:], in0=ein[:], scalar1=top8[:, 1:2], scalar2=None,
                                                              op0=ALU.is_equal), reads=["ein", "top8"], writes=["sel2"])
                          cop("dve", lambda e: e.tensor_tensor(out=rtj["d12"][:], in0=top8[:, 0:1], in1=top8[:, 1:2],
                                                              op=ALU.subtract), reads=["top8"], writes=["rt_d12"])
                          cop("act", lambda e: e.activation(out=rtj["s1"][:], in_=rtj["d12"][:], func=AF.Sigmoid),
                             reads=["rt_d12"], writes=["rt_s1"])
                          cop("act", lambda e: e.activation(out=rtj["s2"][:], in_=rtj["d12"][:], func=AF.Sigmoid, scale=-1.0),
                             reads=["rt_d12"], writes=["rt_s2"])
                          for ohx, selx, kx in ((oh1, sel1, "1"), (oh2, sel2, "2")):
                              cop("dve", lambda e, ohx=ohx, selx=selx: e.tensor_tensor(
                                  out=e3(ohx[:]), in0=gmb, in1=selx[:].unsqueeze(1).to_broadcast([128, NG, EPG]),
                                  op=ALU.mult), reads=["gm", "sel" + kx], writes=["oh" + kx])
                          cop("dve", lambda e: e.tensor_tensor(out=Mt[:], in0=oh1[:], in1=oh2[:], op=ALU.add),
                             reads=["oh1", "oh2"], writes=["Mt"])
                          cop("dve", lambda e: e.tensor_copy(out=Mb[:], in_=Mt[:]), reads=["Mt"], writes=["Mb"])
                          cop("pe", lambda e: e.matmul(ppos[:, 64:64 + c.E], lhsT=ustr[:], rhs=Mb[:], start=True, stop=False),
                             reads=["ustr", "Mb"], writes=[kpos])
                          cop("pe", lambda e: e.matmul(ppos[:, 64:64 + c.E], lhsT=onesb[:], rhs=Mcumb[:], start=False,
                                                      stop=True), reads=["onesb", "Mcumb"], writes=[kpos])
                          cop("dve", lambda e: e.tensor_tensor(out=Mcum[:], in0=Mcum[:], in1=Mt[:], op=ALU.add),
                             reads=["Mcum", "Mt"], writes=["Mcum"])
                          cop("dve", lambda e: e.tensor_copy(out=Mcumb[:], in_=Mcum[:]), reads=["Mcum"], writes=["Mcumb"])
                          posp = ppos[:, 64:64 + c.E]
                          cop("dve", lambda e: e.tensor_scalar(out=valid[:], in0=posp, scalar1=float(C), scalar2=None,
                                                              op0=ALU.is_lt), reads=[kpos], writes=["valid"])
                          cop("dve", lambda e: e.tensor_tensor(out=val[:], in0=posp, in1=ebase[:, 0:E], op=ALU.add),
                             reads=[kpos, "ebase"], writes=["val"])
                          cop("dve", lambda e: e.scalar_tensor_tensor(out=val[:], in0=val[:], scalar=ebase[:, E:E + 1], in1=valid[:],
                                                                     op0=ALU.subtract, op1=ALU.mult),
                             reads=["val", "valid", "ebase"], writes=["val"])
                          cop("dve", lambda e: e.tensor_scalar(out=val[:], in0=val[:], scalar1=ebase[:, E:E + 1], scalar2=None,
                                                              op0=ALU.add), reads=["val", "ebase"], writes=["val"])
                          for ohx, kx, col in ((oh1, "1", 0), (oh2, "2", 1)):
                              cop("dve", lambda e, ohx=ohx: e.tensor_tensor(out=etmp[:], in0=ohx[:], in1=val[:], op=ALU.mult),
                                 reads=["oh" + kx, "val"], writes=["etmp"])
                              cop("dve", lambda e, col=col: e.tensor_reduce(out=slf[:, col:col + 1], in_=etmp[:], axis=AX.X,
                                                                           op=ALU.add), reads=["etmp"], writes=["slf"])
                              cop("dve", lambda e, ohx=ohx: e.tensor_tensor(out=etmp[:], in0=ohx[:], in1=valid[:],
                                                                           op=ALU.mult),
                                 reads=["oh" + kx, "valid"], writes=["etmp"])
                              cop("dve", lambda e, kx=kx: e.tensor_reduce(out=rtj["v" + kx][:], in_=etmp[:], axis=AX.X,
                                                                         op=ALU.add), reads=["etmp"], writes=["rt_v" + kx])
                              cop("dve", lambda e, kx=kx: e.tensor_tensor(out=rtj["g" + kx][:], in0=rtj["gtop"][:],
                                                                         in1=rtj["s" + kx][:], op=ALU.mult),
                                 reads=["rt_gtop", "rt_s" + kx], writes=["rt_g" + kx])
                              cop("dve", lambda e, kx=kx, col=col, jg=jg: e.tensor_tensor(
                                  out=GATES[:, jg, col:col + 1], in0=rtj["g" + kx][:], in1=rtj["v" + kx][:], op=ALU.mult),
                                 reads=["rt_g" + kx, "rt_v" + kx], writes=["GATES"])
                          cop("dve", lambda e, jg=jg: e.tensor_copy(out=SLOTS[:, jg, :], in_=slf[:]), reads=["slf"],
                             writes=["SLOTS"])
                          ck("RT1")
                          for col in range(2):
                              cdma("pool", lambda e, jg=jg, col=col: e.indirect_dma_start(
                                  out=xg, out_offset=bass.IndirectOffsetOnAxis(ap=SLOTS[:, jg, col:col + 1], axis=0),
                                  in_=x1b[:], in_offset=None), "scat",
                                  reads=["x1b", "SLOTS"])
                      OFFS = 6
                      nmax = max(len(ch) for ch in chains) + OFFS * (len(chains) - 1)
                      for s_ in range(nmax):
                          for ci, ch in enumerate(chains):
                              idx = s_ - ci * OFFS
                              if 0 <= idx < len(ch):
                                  kind_, eng_, rec_, dsem_, rd_, wr_ = ch[idx]
                                  if kind_ == "op":
                                      S.commit_op(eng_, rec_, rd_, wr_)
                                  else:
                                      S.commit_dma(eng_, rec_, dsem_, rd_, wr_)
              S.barrier()
              ck("E0")
              with ExitStack() as ee:
                  def se(name, shape, dt=F32):
                      return ee.enter_context(nc.sbuf_tensor("e%d_%s" % (l, name), list(shape), dt))

                  wgb = [se("wgb%d" % i, [128, KD, c.DE], BF16) for i in range(2)]
                  wub = [se("wub%d" % i, [128, KD, c.DE], BF16) for i in range(2)]
                  wdb = [se("wdb%d" % i, [128, KE, D], BF16) for i in range(2)]
                  xgt = [se("xgt%d" % i, [128, D], BF16) for i in range(2 * c.CB)]
                  xT = se("xT", [128, KD, C], BF16)
                  sg = [se("sg%d" % i, [128, C]) for i in range(2)]
                  hT = se("hT", [128, KE, C], BF16)
                  yo = [se("yo%d" % i, [128, D]) for i in range(2)]
                  stage_pool[0] = [se("estage%d" % i, [128, 2048]) for i in range(6)]
                  stage_pool[1] = "estage%d_" % l
                  stage_ctr[0] = 0

                  ecast = [0]
                  deferred = []

                  def load_expert(ei):
                      p = ei % 2
                      for cb in range(c.CB):
                          xi_ = p * c.CB + cb
                          r0 = ei * C + cb * 128
                          dma("sp", lambda e, xi_=xi_, r0=r0: e.dma_start(out=xgt[xi_][:], in_=xg[r0:r0 + 128, :]),
                              "xgt%d" % xi_, writes=["xgt%d" % xi_])
                      for (dst, src, K, F, key) in ((wgb[p], wg_d[l, ei], KD, c.DE, "wgb%d" % p),
                                                    (wub[p], wu_d[l, ei], KD, c.DE, "wub%d" % p),
                                                    (wdb[p], wd_d[l, ei], KE, D, "wdb%d" % p)):
                          sv = src.rearrange("(k p) f -> p k f", p=128)
                          kstep = max(1, 2048 // F)
                          for k0 in range(0, K, kstep):
                              k1 = min(K, k0 + kstep)
                              ceng = ("dve", "pool", "dve", "pool", "dve", "pool")[ecast[0] % 6]
                              ecast[0] += 1
                              load_cast(dst[:, k0:k1, :], sv[:, k0:k1, :], (k1 - k0) * F, key, shape3=(k1 - k0, F), eng=ceng,
                                        defer=(deferred if (ceng != "pool" and ei > 0) else None))

                  load_expert(0)
                  xcnt = 0
                  ycnt = 0
                  for ei in range(E):
                      p = ei % 2
                      if ei + 1 < E:
                          load_expert(ei + 1)
                      for cb in range(c.CB):
                          xi_ = p * c.CB + cb
                          for k0 in range(0, KD, 4):
                              kn = min(4, KD - k0)
                              for kk in range(kn):
                                  op("pe", lambda e, kk=kk, k0=k0, xi_=xi_: e.transpose(
                                      out=psT[:, kk * 128:(kk + 1) * 128],
                                      in_=xgt[xi_][:, (k0 + kk) * 128:(k0 + kk + 1) * 128], identity=identb[:]),
                                     reads=["xgt%d" % xi_, "identb"], writes=["psT"])
                              op("dve", lambda e, k0=k0, kn=kn, cb=cb: e.tensor_copy(
                                  out=xT[:, k0:k0 + kn, cb * 128:(cb + 1) * 128],
                                  in_=psT[:, 0:kn * 128].rearrange("p (k t) -> p k t", k=kn)),
                                 reads=["psT"], writes=["xT"])
                      for fc in range(KE):
                          pg, pu = ps[(fc % 2) * 2], ps[(fc % 2) * 2 + 1]
                          kg, ku = "ps%d" % ((fc % 2) * 2), "ps%d" % ((fc % 2) * 2 + 1)
                          for (pb, pk, wb, wk) in ((pg, kg, wgb[p], "wgb%d" % p), (pu, ku, wub[p], "wub%d" % p)):
                              for kc in range(KD):
                                  op("pe", lambda e, kc=kc, pb=pb, wb=wb, fc=fc: e.matmul(
                                      pb[:, 0:C], lhsT=wb[:, kc, fc * 128:(fc + 1) * 128], rhs=xT[:, kc, :],
                                      start=(kc == 0), stop=(kc == KD - 1)), reads=[wk, "xT"], writes=[pk])
                          si = fc % 2
                          op("act", lambda e, pg=pg, si=si: e.activation(out=sg[si][:], in_=pg[:, 0:C], func=AF.Silu),
                             reads=[kg], writes=["sg%d" % si])
                          op("dve", lambda e, pu=pu, si=si, fc=fc: e.tensor_tensor(out=hT[:, fc, :], in0=pu[:, 0:C],
                                                                                   in1=sg[si][:], op=ALU.mult),
                             reads=[ku, "sg%d" % si], writes=["hT"])
                      for fn_ in deferred:
                          fn_()
                      del deferred[:]
                      for cb in range(c.CB):
                          yi = ycnt % 2
                          ycnt += 1
                          for dh in range(c.NDH):
                              pb, pk = ps[4 + dh % 2], "ps%d" % (4 + dh % 2)
                              for fc in range(KE):
                                  op("pe", lambda e, fc=fc, pb=pb, cb=cb, dh=dh: e.matmul(
                                      pb[:, 0:c.DW], lhsT=hT[:, fc, cb * 128:(cb + 1) * 128],
                                      rhs=wdb[p][:, fc, dh * c.DW:(dh + 1) * c.DW], start=(fc == 0), stop=(fc == KE - 1)),
                                     reads=["hT", "wdb%d" % p], writes=[pk])
                              op("act", lambda e, pb=pb, yi=yi, dh=dh: e.copy(out=yo[yi][:, dh * c.DW:(dh + 1) * c.DW],
                                                                              in_=pb[:, 0:c.DW]),
                                 reads=[pk], writes=["yo%d" % yi])
                          r0 = ei * C + cb * 128
                          dma("sp", lambda e, yi=yi, r0=r0: e.dma_start(out=yg[r0:r0 + 128, :], in_=yo[yi][:]),
                              "yst%d" % yi, reads=["yo%d" % yi])
              S.barrier()
              ck("C0")
              with ExitStack() as ec:
                  def sc(name, shape, dt=F32):
                      return ec.enter_context(nc.sbuf_tensor("c%d_%s" % (l, name), list(shape), dt))

                  rows2 = sc("rows2", [128, 2 * D])
                  y1 = [sc("y1_%d" % i, [128, D]) for i in range(2)]
                  y2 = [sc("y2_%d" % i, [128, D]) for i in range(2)]
                  x1c = [sc("x1c%d" % i, [128, D]) for i in range(2)]
                  z2 = [sc("z2_%d" % i, [128, D]) for i in range(2)]
                  x2 = [sc("x2_%d" % i, [128, D]) for i in range(2)]
                  bst2 = sc("bst2", [128, 2 * c.NDH, 6])
                  mv2 = sc("mv2", [128, 2])
                  rt2 = {nm: sc("rt2_" + nm, [128, 1]) for nm in ("sd", "rstd")}
                  dma("sp", lambda e: e.dma_start(out=rows2[:], in_=rows_d[l][:, 2 * D:4 * D]), "lw2", writes=["rows2"])
                  for i in range(2):
                      op("dve", lambda e, i=i: e.memset(y1[i][:], 0.0), writes=["y1_%d" % i])
                      op("dve", lambda e, i=i: e.memset(y2[i][:], 0.0), writes=["y2_%d" % i])
                  for jg in range(c.NJ):
                      b = jg % 2
                      dma("pool", lambda e, jg=jg, b=b: e.indirect_dma_start(
                          out=y1[b][:], out_offset=None, in_=yg,
                          in_offset=bass.IndirectOffsetOnAxis(ap=SLOTS[:, jg, 0:1], axis=0)), "g1_%d" % b, reads=["SLOTS"], writes=["y1_%d" % b])
                      dma("pool", lambda e, jg=jg, b=b: e.indirect_dma_start(
                          out=y2[b][:], out_offset=None, in_=yg,
                          in_offset=bass.IndirectOffsetOnAxis(ap=SLOTS[:, jg, 1:2], axis=0)), "g2_%d" % b, reads=["SLOTS"], writes=["y2_%d" % b])
                      dma("sp", lambda e, jg=jg, b=b: e.dma_start(out=x1c[b][:], in_=x1s[jg * 128:(jg + 1) * 128, :]),
                          "x1c%d" % b, writes=["x1c%d" % b])
                      op("act", lambda e, b=b: e.mul(out=z2[b][:], in_=x1c[b][:], mul=c.ALPHA), reads=["x1c%d" % b],
                         writes=["z2_%d" % b])
                      op("dve", lambda e, b=b, jg=jg: e.scalar_tensor_tensor(
                          out=z2[b][:], in0=y1[b][:], scalar=GATES[:, jg, 0:1], in1=z2[b][:], op0=ALU.mult, op1=ALU.add),
                         reads=["y1_%d" % b, "GATES", "z2_%d" % b], writes=["z2_%d" % b])
                      op("dve", lambda e, b=b, jg=jg: e.scalar_tensor_tensor(
                          out=z2[b][:], in0=y2[b][:], scalar=GATES[:, jg, 1:2], in1=z2[b][:], op0=ALU.mult, op1=ALU.add),
                         reads=["y2_%d" % b, "GATES", "z2_%d" % b], writes=["z2_%d" % b])
                      layer_norm(c, op, z2[b], x2[b], bst2, mv2, rt2, rows2[:, 0:D], rows2[:, D:2 * D], "z2_%d" % b, "x2_%d" % b,
                                 "rows2", sfx="2")
                      dma("sp", lambda e, jg=jg, b=b: e.dma_start(out=xnext[jg * 128:(jg + 1) * 128, :], in_=x2[b][:]),
                          "x2st%d" % b, reads=["x2_%d" % b])
              S.barrier()
        except _Stop:
            pass
        S.barrier()
        S.emit()
    return nc


def layer_norm(c, op, src, dst, bst, mv, rt, g_row, b_row, ksrc, kdst, krows, sfx=""):
    D = c.D
    nch = 2 * c.NDH if D >= 512 else 1
    w = D // nch
    for i in range(nch):
        op("dve", lambda e, i=i: e.bn_stats(out=bst[:, i, :], in_=src[:, i * w:(i + 1) * w]), reads=[ksrc],
           writes=["bst" + sfx])
    op("dve", lambda e: e.bn_aggr(out=mv[:], in_=bst[:, 0:nch, :].rearrange("p a b -> p (a b)")), reads=["bst" + sfx],
       writes=["mv" + sfx])
    op("act", lambda e: e.activation(out=rt["sd"][:], in_=mv[:, 1:2], func=AF.Sqrt, bias=1e-5), reads=["mv" + sfx],
       writes=["rt_sd" + sfx])
    op("dve", lambda e: e.reciprocal(out=rt["rstd"][:], in_=rt["sd"][:]), reads=["rt_sd" + sfx], writes=["rt_rstd" + sfx])
    op("dve", lambda e: e.tensor_scalar(out=dst[:], in0=src[:], scalar1=mv[:, 0:1], scalar2=rt["rstd"][:],
                                        op0=ALU.subtract, op1=ALU.mult), reads=[ksrc, "mv" + sfx, "rt_rstd" + sfx],
       writes=[kdst])
    op("pool", lambda e: e.tensor_tensor(out=dst[:], in0=dst[:], in1=g_row, op=ALU.mult), reads=[kdst, krows],
       writes=[kdst])
    op("pool", lambda e: e.tensor_tensor(out=dst[:], in0=dst[:], in1=b_row, op=ALU.add), reads=[kdst, krows],
       writes=[kdst])


def prep_inputs(c, inp):
    L = c.L
    f = lambda a: np.ascontiguousarray(np.asarray(a, dtype=np.float32))
    out = {}
    out["w_in"] = f(inp["w_in"])
    out["w_glu"] = f(inp["w_glu"])
    out["w_out"] = f(inp["w_out"])
    KR, KS, GP = c.KR, c.KS, c.GP
    for src, dst in (("lru_wa", "lruA"), ("lru_wx", "lruX")):
        w = f(inp[src])
        blk = np.zeros((L, KR, 128, 128), np.float32)
        for kr in range(KR):
            for h2 in range(2):
                blk[:, kr, h2 * 64:(h2 + 1) * 64, h2 * 64:(h2 + 1) * 64] = w[:, kr * 2 + h2]
        out[dst] = blk
    for src, dst in (("ssm_b_re", "Bre"), ("ssm_b_im", "Bim")):
        b = f(inp[src])
        blk = np.zeros((L, 128, GP, 128), np.float32)
        for ks in range(KS):
            for q in range(4):
                for g2 in range(2):
                    g = ks * 8 + q * 2 + g2
                    p0 = q * 32 + g2 * 16
                    blk[:, p0:p0 + 16, ks * 4 + q, g2 * 64:(g2 + 1) * 64] = np.transpose(b[:, g], (0, 2, 1))
        out[dst] = blk
    for src, dst in (("ssm_c_re", "Cre"), ("ssm_c_im", "Cim")):
        cc = f(inp[src])
        blk = np.zeros((L, 128, GP, 32), np.float32)
        for gp in range(GP):
            for g2 in range(2):
                blk[:, g2 * 64:(g2 + 1) * 64, gp, g2 * 16:(g2 + 1) * 16] = np.transpose(cc[:, 2 * gp + g2], (0, 2, 1))
        out[dst] = blk
    pp = np.zeros((L, 128, c.NPP), np.float32)

    def put(name, arr):
        o, n = c.o[name]
        pp[:, :, o:o + n] = arr

    chan = lambda v, K: np.transpose(f(v).reshape(L, K, 128), (0, 2, 1))
    cw = f(inp["conv_w"])
    cwp = np.transpose(cw.reshape(L, 4, KR, 128), (0, 3, 2, 1)).reshape(L, 128, KR * 4)
    put("convw", cwp)
    put("convb", chan(inp["conv_b"], KR))
    put("ba", chan(inp["lru_ba"], KR))
    put("bx", chan(inp["lru_bx"], KR))
    put("lam", chan(inp["lru_lambda"], KR))
    put("grec", chan(inp["g_rec"], KR))
    put("bglu", chan(inp["b_glu"], 2 * KS))
    put("gssm", chan(inp["g_ssm"], KS))
    put("ssmd", chan(f(inp["ssm_d"]).reshape(L, -1), KS))
    gn = lambda v: np.transpose(f(v).reshape(L, GP, 2 * 64), (0, 2, 1))
    put("lre", gn(inp["ssm_lambda_re"]))
    put("lim", gn(inp["ssm_lambda_im"]))
    ldt = f(inp["ssm_log_dt"]).reshape(L, GP, 2, 1)
    put("ldt", np.transpose(np.broadcast_to(ldt, (L, GP, 2, 64)).reshape(L, GP, 128), (0, 2, 1)))
    out["pp"] = pp
    rows = np.concatenate([f(inp["ln1_g"]), f(inp["ln1_b"]), f(inp["ln2_g"]), f(inp["ln2_b"]),
                           f(inp["router_bg"]), f(inp["router_be"])], axis=1)
    out["rows"] = np.ascontiguousarray(np.broadcast_to(rows[:, None, :], (L, 128, c.NROW)))
    out["wr"] = np.ascontiguousarray(np.concatenate([f(inp["router_wg"]), f(inp["router_we"])], axis=2))
    out["wg"] = f(inp["exp_w_gate"])
    out["wu"] = f(inp["exp_w_up"])
    out["wd"] = f(inp["exp_w_down"])
    out["ident"] = np.eye(128, dtype=np.float32)
    out["ustrict"] = np.triu(np.ones((128, 128), np.float32), 1)
    eb = np.zeros((128, c.E + 1), np.float32)
    eb[:, :c.E] = (np.arange(c.E, dtype=np.float32) * c.C)[None, :]
    eb[:, c.E] = c.E * c.C + np.arange(128, dtype=np.float32)
    out["ebase"] = eb
    return out


_CACHE = {}


def kernel(**inputs):
    c = Cfg()
    x = np.asarray(inputs["x"], dtype=np.float32)
    B = x.shape[0]
    shared = prep_inputs(c, inputs)
    if "nc" not in _CACHE:
        _CACHE["nc"] = build(c)
    nc = _CACHE["nc"]
    in_maps = []
    for b in range(B):
        m = dict(shared)
        m["x"] = np.ascontiguousarray(x[b])
        in_maps.append(m)
    res = run_bass_kernel_spmd(nc, in_maps, core_ids=list(range(B)))
    return np.stack([np.asarray(res.results[b]["y"], dtype=np.float32) for b in range(B)], axis=0)
```

```python
import math
from contextlib import ExitStack

import numpy as np
import concourse.bass as bass
import concourse.mybir as mybir
from concourse.bass_utils import run_bass_kernel_spmd

F32 = mybir.dt.float32
BF16 = mybir.dt.bfloat16
I32 = mybir.dt.int32
AF = mybir.ActivationFunctionType
ALU = mybir.AluOpType
AX = mybir.AxisListType

ENGS = ("pe", "act", "dve", "pool", "sp")


class _Rec:
    def __init__(self):
        self.call = None

    def __getattr__(self, name):
        def f(*a, **k):
            self.call = (name, a, k)
            return self
        return f


def _record(fn):
    r = _Rec()
    fn(r)
    assert r.call is not None
    return r.call


class Sched:
    def __init__(self, nc):
        self.nc = nc
        self.ops = {e: [] for e in ENGS}
        self.seq = {e: 0 for e in ENGS}
        self.waited = {e: {} for e in ENGS}
        self.last_w = {}
        self.readers = {}
        self.dcount = {}
        self.sems = {}
        self.dsem_names = []

    def _deps(self, eng, reads, writes):
        need = {}

        def add(dep):
            if dep is None:
                return
            k, v = dep
            if k.startswith("d:"):
                v = self.dcount[k[2:]]
            if need.get(k, 0) < v:
                need[k] = v

        for b in reads:
            add(self.last_w.get(b))
        for b in writes:
            add(self.last_w.get(b))
            for r in self.readers.get(b, ()):
                add(r)
        out = []
        w = self.waited[eng]
        for k, v in need.items():
            if eng == "pe" and k == "pe":
                continue
            if w.get(k, 0) < v:
                w[k] = v
                out.append((k, v))
        return out

    def _commit(self, token, reads, writes):
        for b in reads:
            self.readers.setdefault(b, []).append(token)
        for b in writes:
            self.last_w[b] = token
            self.readers[b] = []

    enabled = True

    def op(self, eng, fn, reads=(), writes=()):
        if not self.enabled:
            return
        self.commit_op(eng, _record(fn), reads, writes)

    def commit_op(self, eng, rec, reads=(), writes=()):
        if not self.enabled:
            return
        writes = list(writes) + [k for k in reads if k.startswith("ps")]
        reads = [k for k in reads if not k.startswith("ps")]
        waits = self._deps(eng, reads, writes)
        self.seq[eng] += 1
        token = (eng, self.seq[eng])
        self._commit(token, reads, writes)
        self.ops[eng].append(("op", rec, waits, None))

    def dma(self, eng, fn, dsem, reads=(), writes=()):
        if not self.enabled:
            return
        self.commit_dma(eng, _record(fn), dsem, reads, writes)

    def commit_dma(self, eng, rec, dsem, reads=(), writes=()):
        if not self.enabled:
            return
        waits = self._deps(eng, reads, writes)
        if dsem not in self.dcount:
            self.dcount[dsem] = 0
            self.dsem_names.append(dsem)
        self.dcount[dsem] += 16
        token = ("d:" + dsem, self.dcount[dsem])
        self._commit(token, reads, writes)
        self.ops[eng].append(("dma", rec, waits, dsem))

    def barrier(self):
        for e in ENGS:
            waits = []
            w = self.waited[e]
            for o in ENGS:
                if o != e and self.seq[o] > w.get(o, 0):
                    w[o] = self.seq[o]
                    waits.append((o, self.seq[o]))
            for d, c in self.dcount.items():
                k = "d:" + d
                if c > w.get(k, 0):
                    w[k] = c
                    waits.append((k, c))
            self.ops[e].append(("wait", None, waits, None))

    def emit(self):
        nc = self.nc
        with ExitStack() as es:
            for e in ENGS:
                self.sems[e] = es.enter_context(nc.semaphore("s_" + e))
            for d in self.dsem_names:
                self.sems["d:" + d] = es.enter_context(nc.semaphore("d_" + d))
            block = es.enter_context(nc.Block())
            engmap = {"pe": block.tensor, "act": block.scalar, "dve": block.vector,
                      "pool": block.gpsimd, "sp": block.sync}
            for e in ENGS:
                def body(eng, lst=self.ops[e], e=e):
                    regs = {}
                    for kind, fn, waits, dsem in lst:
                        for k, v in waits:
                            eng.wait_ge(self.sems[k], v)
                        if fn is not None and isinstance(fn[2].get("bounds_check"), int):
                            bc = fn[2]["bounds_check"]
                            if bc not in regs:
                                regs[bc] = eng.to_reg(bc)
                            fn = (fn[0], fn[1], dict(fn[2], bounds_check=regs[bc]))
                        if kind == "op":
                            getattr(eng, fn[0])(*fn[1], **fn[2]).then_inc(self.sems[e], 1)
                        elif kind == "dma":
                            getattr(eng, fn[0])(*fn[1], **fn[2]).then_inc(self.sems["d:" + dsem], 16)
                engmap[e](body)


class _Stop(Exception):
    pass


class Cfg:
    stop = None

    def __init__(self, **kw):
        self.L = 4
        self.D = 1024
        self.DR = 512
        self.DS = 512
        self.DE = 512
        self.T = 4096
        self.TT = 256
        self.TS = 128
        self.C = 384
        self.NG = 4
        self.EPG = 8
        for k, v in kw.items():
            setattr(self, k, v)
        c = self
        c.E = c.NG * c.EPG
        c.KD, c.KR, c.KS, c.KE = c.D // 128, c.DR // 128, c.DS // 128, c.DE // 128
        c.GP = c.DS // 32
        c.DP = 2 * c.DR + c.DS
        c.KP = c.DP // 128
        c.JT = c.TT // 128
        c.NTT = c.T // c.TT
        c.NJ = c.T // 128
        c.NST = c.TT // c.TS
        c.CB = c.C // 128
        c.DW = min(c.D, 512)
        c.NDH = c.D // c.DW
        c.NR = c.NG + c.E
        c.ALPHA = (2.0 * c.L) ** 0.25
        o = 0
        c.o = {}
        for name, n in (("convw", c.KR * 4), ("convb", c.KR), ("ba", c.KR), ("bx", c.KR),
                        ("lam", c.KR), ("grec", c.KR), ("bglu", 2 * c.KS), ("gssm", c.KS),
                        ("ssmd", c.KS), ("lre", c.GP), ("lim", c.GP), ("ldt", c.GP)):
            c.o[name] = (o, n)
            o += n
        c.NPP = o
        c.NROW = 4 * c.D + c.NR


def build(c):
    nc = bass.Bass("TRN2", target_bir_lowering=False)
    L, D, T, E, C = c.L, c.D, c.T, c.E, c.C

    def din(name, shape, dt=F32):
        return nc.dram_tensor(name, list(shape), dt, kind="ExternalInput").ap()

    x_in = din("x", [T, D])
    w_in = din("w_in", [L, D, c.DP])
    w_glu = din("w_glu", [L, c.DS, 2 * c.DS])
    w_out = din("w_out", [L, D, D])
    lruA = din("lruA", [L, c.KR, 128, 128])
    lruX = din("lruX", [L, c.KR, 128, 128])
    Bre = din("Bre", [L, 128, c.GP, 128])
    Bim = din("Bim", [L, 128, c.GP, 128])
    Cre = din("Cre", [L, 128, c.GP, 32])
    Cim = din("Cim", [L, 128, c.GP, 32])
    pp_d = din("pp", [L, 128, c.NPP])
    rows_d = din("rows", [L, 128, c.NROW])
    wr_d = din("wr", [L, D, c.NR])
    wg_d = din("wg", [L, E, D, c.DE])
    wu_d = din("wu", [L, E, D, c.DE])
    wd_d = din("wd", [L, E, c.DE, D])
    ident_d = din("ident", [128, 128])
    ustr_d = din("ustrict", [128, 128])
    ebase_d = din("ebase", [128, E + 1])
    y_out = nc.dram_tensor("y", [T, D], F32, kind="ExternalOutput").ap()
    xs = nc.dram_tensor("xs", [T, D], F32, kind="Internal").ap()
    x1s = nc.dram_tensor("x1s", [T, D], F32, kind="Internal").ap()
    xg = nc.dram_tensor("xg", [E * C + 128, D], BF16, kind="Internal").ap()
    yg = nc.dram_tensor("yg", [E * C + 128, D], F32, kind="Internal").ap()

    S = Sched(nc)
    op, dma = S.op, S.dma
    BIG = float(E * C + 64)
    TT, TS, JT = c.TT, c.TS, c.JT
    KD, KR, KS, KE, GP = c.KD, c.KR, c.KS, c.KE, c.GP

    with ExitStack() as es:
        def sb(name, shape, dt=F32):
            return es.enter_context(nc.sbuf_tensor("t_" + name, list(shape), dt))

        def pst(name, shape, dt=F32):
            return es.enter_context(nc.psum_tensor("p_" + name, list(shape), dt))

        ident = sb("ident", [128, 128])
        identb = sb("identb", [128, 128], BF16)
        ustr = sb("ustr", [128, 128], BF16)
        onesb = sb("onesb", [128, 128], BF16)
        ebase = sb("ebase", [128, E + 1])
        stage = [sb("stage%d" % i, [128, 2048]) for i in range(2)]
        SLOTS = sb("SLOTS", [128, c.NJ, 2], I32)
        GATES = sb("GATES", [128, c.NJ, 2])
        ps = [pst("ps%d" % i, [128, 512]) for i in range(7)]
        psT = pst("psT", [128, 1024], BF16)

        stage_ctr = [0]
        stage_pool = [list(stage), "stage"]

        def load_cast(dst_ap, src_ap, ncols, dst_key, shape3=None, eng="pool", defer=None):
            tiles, pfx = stage_pool
            i = stage_ctr[0] % len(tiles)
            stage_ctr[0] += 1
            st = tiles[i]
            sv = st[:, 0:ncols]
            if shape3 is not None:
                sv = sv.rearrange("p (a b) -> p a b", a=shape3[0])
            key = "%s%d" % (pfx, i)
            dma("sp", lambda e: e.dma_start(out=sv, in_=src_ap), key, writes=[key])

            def do_cast():
                if eng == "act":
                    op("act", lambda e: e.copy(out=dst_ap, in_=sv), reads=[key], writes=[dst_key])
                else:
                    op(eng, lambda e: e.tensor_copy(out=dst_ap, in_=sv), reads=[key], writes=[dst_key])

            if defer is not None:
                defer.append(do_cast)
            else:
                do_cast()

        dma("sp", lambda e: e.dma_start(out=ident[:], in_=ident_d), "cst", writes=["ident"])
        dma("sp", lambda e: e.dma_start(out=ebase[:], in_=ebase_d), "cst", writes=["ebase"])
        op("dve", lambda e: e.tensor_copy(out=identb[:], in_=ident[:]), reads=["ident"], writes=["identb"])
        load_cast(ustr[:], ustr_d, 128, "ustr")
        op("dve", lambda e: e.memset(onesb[:], 1.0), writes=["onesb"])
        with nc.sbuf_tensor("t_zt", [128, D], BF16) as zt, nc.sbuf_tensor("t_zf32", [128, D], F32) as zf32:
            op("dve", lambda e: e.memset(zt[:], 0.0), writes=["zt"])
            op("dve", lambda e: e.memset(zf32[:], 0.0), writes=["zf32"])
            dma("sp", lambda e: e.dma_start(out=yg[E * C:E * C + 128, :], in_=zf32[:]), "zf", reads=["zf32"])
            for r0 in range(0, E * C + 128, 128):
                dma("sp", lambda e, r0=r0: e.dma_start(out=xg[r0:r0 + 128, :], in_=zt[:]), "zf",
                    reads=["zt"])
        S.barrier()

        cur_tt = [0]

        def ck(name):
            if c.stop == name or c.stop == "%s@%d" % (name, cur_tt[0]):
                S.enabled = False

        try:
          for l in range(L):
              xcur = x_in if l == 0 else xs
              xnext = y_out if l == L - 1 else xs
              with ExitStack() as em:
                  def sm(name, shape, dt=F32):
                      return em.enter_context(nc.sbuf_tensor("m%d_%s" % (l, name), list(shape), dt))

                  wib = sm("wib", [128, KD, c.DP], BF16)
                  wglub = sm("wglub", [128, KS, 2 * c.DS], BF16)
                  woutb = sm("woutb", [128, KD, D], BF16)
                  lruAb = sm("lruAb", [128, KR, 128], BF16)
                  lruXb = sm("lruXb", [128, KR, 128], BF16)
                  Breb = sm("Breb", [128, GP, 128], BF16)
                  Bimb = sm("Bimb", [128, GP, 128], BF16)
                  Cpre = sm("Cpre", [128, GP, 128], BF16)
                  Cpim = sm("Cpim", [128, GP, 128], BF16)
                  wrf = sm("wrf", [128, KD, c.NR])
                  pp = sm("pp", [128, c.NPP])
                  rows = sm("rows", [128, 2 * D + c.NR])
                  CS = sm("CS", [128, GP, TS])
                  SN = sm("SN", [128, GP, TS])
                  sm_ = {}
                  for nm in ("sp1", "c1", "c2", "lre", "dt", "ldt", "rho", "th", "q", "qf", "thk", "are", "aim",
                             "pre", "den", "t0", "t1", "cre", "cim", "ncim"):
                      sm_[nm] = sm("s_" + nm, [128, max(GP, KR)])
                  qi = sm("s_qi", [128, GP], I32)
                  hcar = sm("hcar", [128, KR])
                  xrc = sm("xrc", [128, GP])
                  xic = sm("xic", [128, GP])
                  cw = [sm("cw%d" % i, [128, 4]) for i in range(4)]
                  Mcum = sm("Mcum", [128, E])
                  Mcumb = sm("Mcumb", [128, E], BF16)
                  xt = [sm("xt%d" % j, [128, D]) for j in range(JT)]
                  xb = sm("xb", [128, D], BF16)
                  xfm = sm("xfm", [128, KD, TT], BF16)
                  gateg = sm("gateg", [128, KR, TT])
                  recin = sm("recin", [128, KR, TT + 3])
                  uf = sm("uf", [128, KS, TT])
                  ub = sm("ub", [128, KS, TT], BF16)
                  rw = {nm: sm("rw_" + nm, [128, TT]) for nm in ("rc", "r", "i", "a", "mu", "bt", "h", "yr")}
                  rcb = sm("rcb", [128, TT], BF16)
                  sw = {nm: sm("sw_" + nm, [128, 4 * TS]) for nm in
                        ("ta", "tb", "br", "bi", "wr0", "wi0", "wr1", "wi1", "ma", "mb")}
                  assert GP * 32 <= 4 * TS
                  cview = lambda t: t[:, 0:GP * 32].rearrange("p (g m) -> p g m", g=GP)
                  Cre_f, Cim_f = cview(sw["ta"]), cview(sw["tb"])
                  ctmp = [cview(sw["ma"]), cview(sw["mb"])]
                  xrb = [sm("xrb%d" % i, [128, 4 * TS], BF16) for i in range(2)]
                  xib = [sm("xib%d" % i, [128, 4 * TS], BF16) for i in range(2)]
                  ysq = sm("ysq", [128, KD, TT], BF16)
                  ybf = sm("ybf", [128, KD, TT], BF16)
                  gy = sm("gy", [128, KS, TT], BF16)
                  sgl = sm("sgl", [128, TT])
                  yss = sm("yss", [128, TT])
                  RB = []
                  for j_ in range(JT):
                      d_ = {}
                      for nm, shp, dt_ in (("lg", [128, c.NR], F32), ("exg", [128, c.NG], F32), ("gm", [128, c.NG], F32),
                                           ("etmp", [128, E], F32), ("ein", [128, c.EPG], F32), ("top8", [128, 8], F32),
                                           ("sel1", [128, c.EPG], F32), ("sel2", [128, c.EPG], F32), ("oh1", [128, E], F32),
                                           ("oh2", [128, E], F32), ("Mt", [128, E], F32), ("Mb", [128, E], BF16),
                                           ("valid", [128, E], F32), ("val", [128, E], F32), ("slf", [128, 2], F32),
                                           ("x1b", [128, D], BF16), ("zz", [128, D], F32), ("x1T", [128, KD, 128], F32),
                                           ("bst", [128, 2 * c.NDH, 6], F32),
                                           ("mv", [128, 2], F32)):
                          d_[nm] = sm("rb%d_%s" % (j_, nm), shp, dt_)
                      d_["rt"] = {nm: sm("rb%d_rt_%s" % (j_, nm), [128, 1]) for nm in
                                  ("gmax", "ngmax", "gsum", "gtop", "d12", "s1", "s2", "v1", "v2", "g1", "g2",
                                   "ssr", "sss", "rsr", "rss", "sd", "rstd")}
                      RB.append(d_)
                  CHAIN_KEYS = set(["lg", "exg", "gm", "etmp", "ein", "top8", "sel1", "sel2", "oh1", "oh2", "Mt", "Mb",
                                    "valid", "val", "slf", "x1b"] + ["rt_" + n for n in
                                   ("gmax", "ngmax", "gsum", "gtop", "d12", "s1", "s2", "v1", "v2", "g1", "g2",
                                    "ssr", "sss", "rsr", "rss", "sd", "rstd")] + ["zz", "x1T", "bst", "mv"])

                  def P(name, i=None):
                      o, n = c.o[name]
                      if i is None:
                          return pp[:, o:o + n]
                      return pp[:, o + i:o + i + 1]

                  stage_pool[0] = list(stage)
                  stage_pool[1] = "stage"
                  stage_ctr[0] = 0
                  dma("sp", lambda e: e.dma_start(out=pp[:], in_=pp_d[l]), "lw", writes=["pp"])
                  dma("sp", lambda e: e.dma_start(out=rows[:, 0:2 * D], in_=rows_d[l][:, 0:2 * D]), "lw", writes=["rows"])
                  dma("sp", lambda e: e.dma_start(out=rows[:, 2 * D:2 * D + c.NR], in_=rows_d[l][:, 4 * D:4 * D + c.NR]),
                      "lw", writes=["rows"])
                  dma("sp", lambda e: e.dma_start(out=wrf[:], in_=wr_d[l].rearrange("(k p) n -> p k n", p=128)), "lw",
                      writes=["wrf"])
                  dma("sp", lambda e: e.dma_start(out=Cre_f, in_=Cre[l]), "lw", writes=["sw_ta"])
                  dma("sp", lambda e: e.dma_start(out=Cim_f, in_=Cim[l]), "lw", writes=["sw_tb"])
                  win_v = w_in[l].rearrange("(k p) f -> p k f", p=128)
                  fstep = 2048 // 512 * 512 if c.DP >= 512 else c.DP
                  for kc in range(KD):
                      for f0 in range(0, c.DP, 2048):
                          f1 = min(c.DP, f0 + 2048)
                          load_cast(wib[:, kc, f0:f1], win_v[:, kc, f0:f1], f1 - f0, "wib")
                  wgl_v = w_glu[l].rearrange("(k p) f -> p k f", p=128)
                  for kc in range(KS):
                      load_cast(wglub[:, kc, :], wgl_v[:, kc, :], 2 * c.DS, "wglub")
                  wo_v = w_out[l].rearrange("(k p) f -> p k f", p=128)
                  for kc in range(KD):
                      load_cast(woutb[:, kc, :], wo_v[:, kc, :], D, "woutb")
                  for kr in range(KR):
                      load_cast(lruAb[:, kr, :], lruA[l, kr], 128, "lruAb")
                      load_cast(lruXb[:, kr, :], lruX[l, kr], 128, "lruXb")
                  for g0 in range(0, GP, 16):
                      g1 = min(GP, g0 + 16)
                      load_cast(Breb[:, g0:g1, :], Bre[l][:, g0:g1, :], (g1 - g0) * 128, "Breb", shape3=(g1 - g0, 128))
                      load_cast(Bimb[:, g0:g1, :], Bim[l][:, g0:g1, :], (g1 - g0) * 128, "Bimb", shape3=(g1 - g0, 128))
                  op("pool", lambda e: e.memset(Cpre[:], 0.0), writes=["Cpre"])
                  op("pool", lambda e: e.memset(Cpim[:], 0.0), writes=["Cpim"])

                  g = lambda nm, n=GP: sm_[nm][:, 0:n]
                  op("act", lambda e: e.activation(out=g("sp1", KR), in_=P("lam"), func=AF.Exp, scale=-1.0),
                     reads=["pp"], writes=["s_sp1"])
                  op("act", lambda e: e.activation(out=g("sp1", KR), in_=g("sp1", KR), func=AF.Ln, bias=1.0),
                     reads=["s_sp1"], writes=["s_sp1"])
                  op("dve", lambda e: e.tensor_scalar(out=g("c1", KR), in0=g("sp1", KR), scalar1=-8.0, scalar2=None,
                                                      op0=ALU.mult), reads=["s_sp1"], writes=["s_c1"])
                  op("dve", lambda e: e.tensor_scalar(out=g("c2", KR), in0=g("sp1", KR), scalar1=-16.0, scalar2=None,
                                                      op0=ALU.mult), reads=["s_sp1"], writes=["s_c2"])
                  op("dve", lambda e: e.tensor_scalar(out=g("lre"), in0=P("lre"), scalar1=-1e-4, scalar2=None,
                                                      op0=ALU.min), reads=["pp"], writes=["s_lre"])
                  op("act", lambda e: e.activation(out=g("dt"), in_=P("ldt"), func=AF.Exp), reads=["pp"], writes=["s_dt"])
                  op("dve", lambda e: e.tensor_tensor(out=g("ldt"), in0=g("lre"), in1=g("dt"), op=ALU.mult),
                     reads=["s_lre", "s_dt"], writes=["s_ldt"])
                  op("act", lambda e: e.activation(out=g("rho"), in_=g("ldt"), func=AF.Exp), reads=["s_ldt"],
                     writes=["s_rho"])
                  op("dve", lambda e: e.tensor_tensor(out=g("th"), in0=P("lim"), in1=g("dt"), op=ALU.mult),
                     reads=["pp", "s_dt"], writes=["s_th"])
                  op("dve", lambda e: e.tensor_scalar(out=g("q"), in0=g("th"), scalar1=1.0 / (2 * math.pi), scalar2=None,
                                                      op0=ALU.mult), reads=["s_th"], writes=["s_q"])
                  op("dve", lambda e: e.tensor_copy(out=qi[:], in_=g("q")), reads=["s_q"], writes=["s_qi"])
                  op("dve", lambda e: e.tensor_copy(out=g("qf"), in_=qi[:]), reads=["s_qi"], writes=["s_qf"])
                  op("dve", lambda e: e.scalar_tensor_tensor(out=g("thk"), in0=g("qf"), scalar=-2 * math.pi, in1=g("th"),
                                                             op0=ALU.mult, op1=ALU.add),
                     reads=["s_qf", "s_th"], writes=["s_thk"])
                  def wrap(ap, key, scr, skey):
                      op("dve", lambda e: e.tensor_scalar(out=scr, in0=ap, scalar1=math.pi, scalar2=None, op0=ALU.is_gt),
                         reads=[key], writes=[skey])
                      op("dve", lambda e: e.scalar_tensor_tensor(out=ap, in0=scr, scalar=-2 * math.pi, in1=ap,
                                                                 op0=ALU.mult, op1=ALU.add), reads=[skey, key], writes=[key])
                      op("dve", lambda e: e.tensor_scalar(out=scr, in0=ap, scalar1=-math.pi, scalar2=None, op0=ALU.is_lt),
                         reads=[key], writes=[skey])
                      op("dve", lambda e: e.scalar_tensor_tensor(out=ap, in0=scr, scalar=2 * math.pi, in1=ap,
                                                                 op0=ALU.mult, op1=ALU.add), reads=[skey, key], writes=[key])

                  wrap(g("thk"), "s_thk", g("t0"), "s_t0")
                  op("dve", lambda e: e.tensor_copy(out=CS[:, :, 0:1], in_=sm_["thk"][:, 0:GP].unsqueeze(2)),
                     reads=["s_thk"], writes=["CS"])
                  w = 1
                  while w < TS:
                      op("dve", lambda e, w=w: e.tensor_tensor(
                          out=CS[:, :, w:2 * w], in0=CS[:, :, 0:w],
                          in1=sm_["thk"][:, 0:GP].unsqueeze(2).to_broadcast([128, GP, w]), op=ALU.add),
                         reads=["CS", "s_thk"], writes=["CS"])
                      wrap(CS[:, :, w:2 * w], "CS", SN[:, :, w:2 * w], "SN")
                      op("dve", lambda e: e.tensor_tensor(out=g("thk"), in0=g("thk"), in1=g("thk"), op=ALU.add),
                         reads=["s_thk"], writes=["s_thk"])
                      wrap(g("thk"), "s_thk", g("t0"), "s_t0")
                      w *= 2
                  op("act", lambda e: e.activation(out=SN[:], in_=CS[:], func=AF.Sin), reads=["CS"], writes=["SN"])
                  op("dve", lambda e: e.scalar_tensor_tensor(out=CS[:], in0=CS[:], scalar=-1.0, in1=CS[:], op0=ALU.mult,
                                                             op1=ALU.max), reads=["CS", "SN"], writes=["CS"])
                  op("act", lambda e: e.activation(out=CS[:], in_=CS[:], func=AF.Sin, scale=-1.0, bias=math.pi / 2),
                     reads=["CS"], writes=["CS"])
                  cs0 = CS[:, :, 0:1].rearrange("p g o -> p (g o)")
                  sn0 = SN[:, :, 0:1].rearrange("p g o -> p (g o)")
                  op("dve", lambda e: e.tensor_tensor(out=g("are"), in0=g("rho"), in1=cs0, op=ALU.mult),
                     reads=["s_rho", "CS"], writes=["s_are"])
                  op("dve", lambda e: e.tensor_tensor(out=g("aim"), in0=g("rho"), in1=sn0, op=ALU.mult),
                     reads=["s_rho", "SN"], writes=["s_aim"])
                  op("dve", lambda e: e.tensor_scalar(out=g("pre"), in0=g("are"), scalar1=-1.0, scalar2=None, op0=ALU.add),
                     reads=["s_are"], writes=["s_pre"])
                  op("dve", lambda e: e.tensor_tensor(out=g("den"), in0=g("lre"), in1=g("lre"), op=ALU.mult),
                     reads=["s_lre"], writes=["s_den"])
                  op("dve", lambda e: e.tensor_tensor(out=g("t0"), in0=P("lim"), in1=P("lim"), op=ALU.mult),
                     reads=["pp"], writes=["s_t0"])
                  op("dve", lambda e: e.tensor_tensor(out=g("den"), in0=g("den"), in1=g("t0"), op=ALU.add),
                     reads=["s_den", "s_t0"], writes=["s_den"])
                  op("dve", lambda e: e.reciprocal(out=g("den"), in_=g("den")), reads=["s_den"], writes=["s_den"])
                  op("dve", lambda e: e.tensor_tensor(out=g("t0"), in0=g("pre"), in1=g("lre"), op=ALU.mult),
                     reads=["s_pre", "s_lre"], writes=["s_t0"])
                  op("dve", lambda e: e.tensor_tensor(out=g("t1"), in0=g("aim"), in1=P("lim"), op=ALU.mult),
                     reads=["s_aim", "pp"], writes=["s_t1"])
                  op("dve", lambda e: e.tensor_tensor(out=g("t0"), in0=g("t0"), in1=g("t1"), op=ALU.add),
                     reads=["s_t0", "s_t1"], writes=["s_t0"])
                  op("dve", lambda e: e.tensor_tensor(out=g("cre"), in0=g("t0"), in1=g("den"), op=ALU.mult),
                     reads=["s_t0", "s_den"], writes=["s_cre"])
                  op("dve", lambda e: e.tensor_tensor(out=g("t0"), in0=g("aim"), in1=g("lre"), op=ALU.mult),
                     reads=["s_aim", "s_lre"], writes=["s_t0"])
                  op("dve", lambda e: e.tensor_tensor(out=g("t1"), in0=g("pre"), in1=P("lim"), op=ALU.mult),
                     reads=["s_pre", "pp"], writes=["s_t1"])
                  op("dve", lambda e: e.tensor_tensor(out=g("t0"), in0=g("t0"), in1=g("t1"), op=ALU.subtract),
                     reads=["s_t0", "s_t1"], writes=["s_t0"])
                  op("dve", lambda e: e.tensor_tensor(out=g("cim"), in0=g("t0"), in1=g("den"), op=ALU.mult),
                     reads=["s_t0", "s_den"], writes=["s_cim"])
                  creb = sm_["cre"][:, 0:GP].unsqueeze(2).to_broadcast([128, GP, 32])
                  cimb = sm_["cim"][:, 0:GP].unsqueeze(2).to_broadcast([128, GP, 32])
                  op("dve", lambda e: e.tensor_tensor(out=ctmp[0], in0=Cre_f, in1=creb, op=ALU.mult),
                     reads=["sw_ta", "s_cre"], writes=["sw_ma"])
                  op("dve", lambda e: e.tensor_tensor(out=ctmp[1], in0=Cim_f, in1=cimb, op=ALU.mult),
                     reads=["sw_tb", "s_cim"], writes=["sw_mb"])
                  w4 = lambda ap, q, lo, hi: ap.rearrange("p (k q) m -> p k q m", q=4)[:, :, q, lo:hi]
                  for q4 in range(4):
                      op("dve", lambda e, q4=q4: e.tensor_tensor(
                          out=w4(Cpre[:], q4, q4 * 32, (q4 + 1) * 32), in0=w4(ctmp[0], q4, 0, 32),
                          in1=w4(ctmp[1], q4, 0, 32), op=ALU.subtract), reads=["sw_ma", "sw_mb"], writes=["Cpre"])
                  op("dve", lambda e: e.tensor_tensor(out=ctmp[0], in0=Cre_f, in1=cimb, op=ALU.mult),
                     reads=["sw_ta", "s_cim", "Cpre"], writes=["sw_ma"])
                  op("dve", lambda e: e.tensor_tensor(out=ctmp[1], in0=Cim_f, in1=creb, op=ALU.mult),
                     reads=["sw_tb", "s_cre", "Cpre"], writes=["sw_mb"])
                  for q4 in range(4):
                      op("dve", lambda e, q4=q4: e.scalar_tensor_tensor(
                          out=w4(Cpim[:], q4, q4 * 32, (q4 + 1) * 32), in0=w4(ctmp[0], q4, 0, 32), scalar=-1.0,
                          in1=w4(ctmp[1], q4, 0, 32), op0=ALU.mult, op1=ALU.subtract),
                         reads=["sw_ma", "sw_mb"], writes=["Cpim"])
                  op("dve", lambda e: e.memset(hcar[:], 0.0), writes=["hcar"])
                  op("dve", lambda e: e.memset(xrc[:], 0.0), writes=["xrc"])
                  op("dve", lambda e: e.memset(xic[:], 0.0), writes=["xic"])
                  op("dve", lambda e: e.memset(recin[:], 0.0), writes=["recin"])
                  op("dve", lambda e: e.memset(Mcum[:], 0.0), writes=["Mcum"])
                  op("dve", lambda e: e.memset(Mcumb[:], 0.0), writes=["Mcumb"])

                  ck("setup")
                  for tt in range(c.NTT):
                      t0 = tt * TT
                      cur_tt[0] = tt
                      if tt == 1:
                          ck("T1")
                      for j in range(JT):
                          dma("sp", lambda e, j=j: e.dma_start(out=xt[j][:], in_=xcur[t0 + j * 128:t0 + (j + 1) * 128, :]),
                              "xt%d" % j, writes=["xt%d" % j])
                          op("pool", lambda e, j=j: e.tensor_copy(out=xb[:], in_=xt[j][:]), reads=["xt%d" % j],
                             writes=["xb"])
                          for k0 in range(0, KD, 4):
                              kn = min(4, KD - k0)
                              for kk in range(kn):
                                  op("pe", lambda e, kk=kk, k0=k0: e.transpose(
                                      out=psT[:, kk * 128:(kk + 1) * 128],
                                      in_=xb[:, (k0 + kk) * 128:(k0 + kk + 1) * 128], identity=identb[:]),
                                     reads=["xb", "identb"], writes=["psT"])
                              op("act", lambda e, k0=k0, kn=kn, j=j: e.copy(
                                  out=xfm[:, k0:k0 + kn, j * 128:(j + 1) * 128],
                                  in_=psT[:, 0:kn * 128].rearrange("p (k t) -> p k t", k=kn)),
                                 reads=["psT"], writes=["xfm"])
                      ck("M2")
                      for fc in range(c.KP):
                          ck("M3f%d" % fc)
                          pb = ps[fc % 2]
                          pk = "ps%d" % (fc % 2)
                          for kc in range(KD):
                              op("pe", lambda e, fc=fc, kc=kc, pb=pb: e.matmul(
                                  pb[:, 0:TT], lhsT=wib[:, kc, fc * 128:(fc + 1) * 128], rhs=xfm[:, kc, :],
                                  start=(kc == 0), stop=(kc == KD - 1)), reads=["wib", "xfm"], writes=[pk])
                          if fc < KR:
                              op("act", lambda e, fc=fc, pb=pb: e.activation(out=gateg[:, fc, :], in_=pb[:, 0:TT],
                                                                              func=AF.Gelu), reads=[pk], writes=["gateg"])
                          elif fc < 2 * KR:
                              op("act", lambda e, fc=fc, pb=pb: e.copy(out=recin[:, fc - KR, 3:3 + TT], in_=pb[:, 0:TT]),
                                 reads=[pk], writes=["recin"])
                          else:
                              op("act", lambda e, fc=fc, pb=pb: e.copy(out=uf[:, fc - 2 * KR, :], in_=pb[:, 0:TT]),
                                 reads=[pk], writes=["uf"])
                              op("dve", lambda e, fc=fc, pb=pb: e.tensor_copy(out=ub[:, fc - 2 * KR, :], in_=pb[:, 0:TT]),
                                 reads=[pk], writes=["ub"])
                      ck("M3")
                      def rec_a(kr):
                          rc, r_, i_, a_, mu, bt, h_, yr = (rw[n] for n in ("rc", "r", "i", "a", "mu", "bt", "h", "yr"))
                          cwo = c.o["convw"][0] + kr * 4
                          op("dve", lambda e, kr=kr, cwo=cwo: e.tensor_scalar(
                              out=rc[:], in0=recin[:, kr, 0:TT], scalar1=pp[:, cwo:cwo + 1], scalar2=P("convb", kr),
                              op0=ALU.mult, op1=ALU.add), reads=["recin", "pp"], writes=["rw_rc"])
                          for k in range(1, 4):
                              op("dve", lambda e, kr=kr, k=k, cwo=cwo: e.scalar_tensor_tensor(
                                  out=rc[:], in0=recin[:, kr, k:k + TT], scalar=pp[:, cwo + k:cwo + k + 1], in1=rc[:],
                                  op0=ALU.mult, op1=ALU.add), reads=["recin", "pp", "rw_rc"], writes=["rw_rc"])
                          op("pool", lambda e: e.tensor_copy(out=rcb[:], in_=rc[:]), reads=["rw_rc"], writes=["rcb"])
                          op("pe", lambda e, kr=kr: e.matmul(ps[3][:, 0:TT], lhsT=lruAb[:, kr, :], rhs=rcb[:],
                                                             start=True, stop=True), reads=["lruAb", "rcb"], writes=["ps3"])
                          op("pe", lambda e, kr=kr: e.matmul(ps[3][:, TT:2 * TT], lhsT=lruXb[:, kr, :], rhs=rcb[:],
                                                             start=True, stop=True), reads=["lruXb", "rcb"], writes=["ps3"])
                          op("act", lambda e, kr=kr: e.activation(out=r_[:], in_=ps[3][:, 0:TT], func=AF.Sigmoid,
                                                                  bias=P("ba", kr)), reads=["ps3", "pp"], writes=["rw_r"])
                          op("act", lambda e, kr=kr: e.activation(out=i_[:], in_=ps[3][:, TT:2 * TT], func=AF.Sigmoid,
                                                                  bias=P("bx", kr)), reads=["ps3", "pp"], writes=["rw_i"])
                          op("act", lambda e, kr=kr: e.activation(out=a_[:], in_=r_[:], func=AF.Exp,
                                                                  scale=sm_["c1"][:, kr:kr + 1]),
                             reads=["rw_r", "s_c1"], writes=["rw_a"])
                          op("act", lambda e, kr=kr: e.activation(out=mu[:], in_=r_[:], func=AF.Exp,
                                                                  scale=sm_["c2"][:, kr:kr + 1]),
                             reads=["rw_r", "s_c2"], writes=["rw_mu"])
                          op("act", lambda e: e.activation(out=mu[:], in_=mu[:], func=AF.Sqrt, scale=-1.0, bias=1.0),
                             reads=["rw_mu"], writes=["rw_mu"])
                          op("pool", lambda e: e.tensor_tensor(out=bt[:], in0=i_[:], in1=rc[:], op=ALU.mult),
                             reads=["rw_i", "rw_rc"], writes=["rw_bt"])
                          op("pool", lambda e: e.tensor_tensor(out=bt[:], in0=bt[:], in1=mu[:], op=ALU.mult),
                             reads=["rw_bt", "rw_mu"], writes=["rw_bt"])
                      def rec_b(kr):
                          rc, r_, i_, a_, mu, bt, h_, yr = (rw[n] for n in ("rc", "r", "i", "a", "mu", "bt", "h", "yr"))
                          op("dve", lambda e, kr=kr: e.tensor_tensor_scan(
                              out=h_[:], data0=a_[:], data1=bt[:], initial=hcar[:, kr:kr + 1], op0=ALU.mult, op1=ALU.add),
                             reads=["rw_a", "rw_bt", "hcar"], writes=["rw_h"])
                          op("dve", lambda e, kr=kr: e.tensor_copy(out=hcar[:, kr:kr + 1], in_=h_[:, TT - 1:TT]),
                             reads=["rw_h"], writes=["hcar"])
                          op("dve", lambda e, kr=kr: e.tensor_tensor(out=yr[:], in0=gateg[:, kr, :], in1=h_[:],
                                                                     op=ALU.mult), reads=["gateg", "rw_h"], writes=["rw_yr"])
                          op("act", lambda e, kr=kr: e.activation(out=ysq[:, kr, :], in_=yr[:], func=AF.Square),
                             reads=["rw_yr"], writes=["ysq"])
                          op("pool", lambda e, kr=kr: e.tensor_scalar(out=ybf[:, kr, :], in0=yr[:], scalar1=P("grec", kr),
                                                                      scalar2=0.0, op0=ALU.mult, op1=ALU.add),
                             reads=["rw_yr", "pp"], writes=["ybf"])
                          op("pool", lambda e, kr=kr: e.tensor_copy(out=recin[:, kr, 0:3], in_=recin[:, kr, TT:TT + 3]),
                             reads=["recin", "rw_rc"], writes=["recin"])
                      ck("M4")
                      W4 = 4 * TS
                      f4 = lambda ap: ap.rearrange("p g t -> p (g t)")
                      g4 = lambda ap: ap.rearrange("p (g t) -> p g t", g=4)
                      if True:
                          s5_late = []

                          def s5_carry(ks, par, WR, WI, kwr, kwi, gsl):
                              wrl = g4(WR[:])[:, :, TS - 1:TS].rearrange("p g o -> p (g o)")
                              wil = g4(WI[:])[:, :, TS - 1:TS].rearrange("p g o -> p (g o)")
                              csl = CS[:, gsl, TS - 1:TS].rearrange("p g o -> p (g o)")
                              snl = SN[:, gsl, TS - 1:TS].rearrange("p g o -> p (g o)")
                              op("dve", lambda e: e.tensor_tensor(out=cw[0][:], in0=wrl, in1=csl, op=ALU.mult),
                                 reads=[kwr, "CS"], writes=["cw0"])
                              op("dve", lambda e: e.tensor_tensor(out=cw[1][:], in0=wil, in1=snl, op=ALU.mult),
                                 reads=[kwi, "SN"], writes=["cw1"])
                              op("dve", lambda e: e.tensor_tensor(out=cw[2][:], in0=wil, in1=csl, op=ALU.mult),
                                 reads=[kwi, "CS"], writes=["cw2"])
                              op("dve", lambda e: e.tensor_tensor(out=cw[3][:], in0=wrl, in1=snl, op=ALU.mult),
                                 reads=[kwr, "SN"], writes=["cw3"])
                              op("dve", lambda e: e.tensor_tensor(out=xrc[:, gsl], in0=cw[0][:], in1=cw[1][:],
                                                                  op=ALU.subtract), reads=["cw0", "cw1"], writes=["xrc"])
                              op("dve", lambda e: e.tensor_tensor(out=xic[:, gsl], in0=cw[2][:], in1=cw[3][:],
                                                                  op=ALU.add), reads=["cw2", "cw3"], writes=["xic"])

                          def s5_iter(st, ks, par):
                              pvr, kvr = (ps[4], "ps4") if par == 0 else (ps[0], "ps0")
                              pvi, kvi = (ps[5], "ps5") if par == 0 else (ps[1], "ps1")
                              py, kpy = (ps[6], "ps6") if par == 0 else (ps[2], "ps2")
                              tsl = slice(st * TS, (st + 1) * TS)
                              for q in range(4):
                                  gp = ks * 4 + q
                                  op("pe", lambda e, gp=gp, q=q, pvr=pvr: e.matmul(
                                      pvr[:, q * TS:(q + 1) * TS], lhsT=Breb[:, gp, :], rhs=ub[:, ks, tsl],
                                      start=True, stop=True), reads=["Breb", "ub"], writes=[kvr])
                              for q in range(4):
                                  gp = ks * 4 + q
                                  op("pe", lambda e, gp=gp, q=q, pvi=pvi: e.matmul(
                                      pvi[:, q * TS:(q + 1) * TS], lhsT=Bimb[:, gp, :], rhs=ub[:, ks, tsl],
                                      start=True, stop=True), reads=["Bimb", "ub"], writes=[kvi])
                              CS4 = f4(CS[:, ks * 4:(ks + 1) * 4, :])
                              SN4 = f4(SN[:, ks * 4:(ks + 1) * 4, :])
                              TA, TB, BR, BI = sw["ta"], sw["tb"], sw["br"], sw["bi"]
                              WR, WI = sw["wr%d" % par], sw["wi%d" % par]
                              kwr, kwi = "sw_wr%d" % par, "sw_wi%d" % par
                              op("dve", lambda e: e.tensor_tensor(out=TA[:], in0=pvr[:, 0:W4], in1=CS4, op=ALU.mult),
                                 reads=[kvr, "CS"], writes=["sw_ta"])
                              op("dve", lambda e: e.tensor_tensor(out=TB[:], in0=pvi[:, 0:W4], in1=SN4, op=ALU.mult),
                                 reads=[kvi, "SN"], writes=["sw_tb"])
                              op("dve", lambda e: e.tensor_tensor(out=BR[:], in0=TA[:], in1=TB[:], op=ALU.add),
                                 reads=["sw_ta", "sw_tb"], writes=["sw_br"])
                              op("dve", lambda e: e.tensor_tensor(out=TA[:], in0=pvi[:, 0:W4], in1=CS4, op=ALU.mult),
                                 reads=[kvi, "CS", "sw_br"], writes=["sw_ta"])
                              op("dve", lambda e: e.tensor_tensor(out=TB[:], in0=pvr[:, 0:W4], in1=SN4, op=ALU.mult),
                                 reads=[kvr, "SN", "sw_br"], writes=["sw_tb"])
                              op("dve", lambda e: e.tensor_tensor(out=BI[:], in0=TA[:], in1=TB[:], op=ALU.subtract),
                                 reads=["sw_ta", "sw_tb"], writes=["sw_bi"])
                              for q in range(4):
                                  gp = ks * 4 + q
                                  qs = slice(q * TS, (q + 1) * TS)
                                  rho_b = sm_["rho"][:, gp:gp + 1].to_broadcast([128, TS])
                                  op("dve", lambda e, gp=gp, qs=qs, rho_b=rho_b: e.tensor_tensor_scan(
                                      out=WR[:, qs], data0=rho_b, data1=BR[:, qs], initial=xrc[:, gp:gp + 1],
                                      op0=ALU.mult, op1=ALU.add), reads=["sw_br", "xrc", "s_rho"], writes=[kwr])
                                  op("dve", lambda e, gp=gp, qs=qs, rho_b=rho_b: e.tensor_tensor_scan(
                                      out=WI[:, qs], data0=rho_b, data1=BI[:, qs], initial=xic[:, gp:gp + 1],
                                      op0=ALU.mult, op1=ALU.add), reads=["sw_bi", "xic", "s_rho"], writes=[kwi])
                              prev_late = list(s5_late)
                              del s5_late[:]
                              for fn_ in prev_late:
                                  fn_()
                              gsl = slice(ks * 4, (ks + 1) * 4)
                              if KS > 1:
                                  s5_late.append(lambda: s5_carry(ks, par, WR, WI, kwr, kwi, gsl))
                              else:
                                  s5_carry(ks, par, WR, WI, kwr, kwi, gsl)
                              MA, MB = sw["ma"], sw["mb"]
                              XR, XI = xrb[par], xib[par]
                              op("pool", lambda e: e.tensor_tensor(out=MA[:], in0=WR[:], in1=CS4, op=ALU.mult),
                                 reads=[kwr, "CS"], writes=["sw_ma"])
                              op("pool", lambda e: e.tensor_tensor(out=MB[:], in0=WI[:], in1=SN4, op=ALU.mult),
                                 reads=[kwi, "SN"], writes=["sw_mb"])
                              op("pool", lambda e: e.tensor_tensor(out=XR[:], in0=MA[:], in1=MB[:], op=ALU.subtract),
                                 reads=["sw_ma", "sw_mb"], writes=["xrb%d" % par])
                              op("pool", lambda e: e.tensor_tensor(out=MA[:], in0=WI[:], in1=CS4, op=ALU.mult),
                                 reads=[kwi, "CS", "xrb%d" % par], writes=["sw_ma"])
                              op("pool", lambda e: e.tensor_tensor(out=MB[:], in0=WR[:], in1=SN4, op=ALU.mult),
                                 reads=[kwr, "SN", "xrb%d" % par], writes=["sw_mb"])
                              op("pool", lambda e: e.tensor_tensor(out=XI[:], in0=MA[:], in1=MB[:], op=ALU.add),
                                 reads=["sw_ma", "sw_mb"], writes=["xib%d" % par])
                              for q in range(4):
                                  gp = ks * 4 + q
                                  qs = slice(q * TS, (q + 1) * TS)
                                  op("pe", lambda e, gp=gp, q=q, qs=qs, py=py: e.matmul(
                                      py[:, 0:TS], lhsT=Cpre[:, gp, :], rhs=XR[:, qs], start=(q == 0), stop=False),
                                     reads=["Cpre", "xrb%d" % par], writes=[kpy])
                                  op("pe", lambda e, gp=gp, q=q, qs=qs, py=py: e.matmul(
                                      py[:, 0:TS], lhsT=Cpim[:, gp, :], rhs=XI[:, qs], start=False, stop=(q == 3)),
                                     reads=["Cpim", "xib%d" % par], writes=[kpy])
                              s5_late.append(lambda: op("dve", lambda e, ks=ks, py=py: e.scalar_tensor_tensor(
                                  out=uf[:, ks, tsl], in0=uf[:, ks, tsl], scalar=P("ssmd", ks), in1=py[:, 0:TS],
                                  op0=ALU.mult, op1=ALU.add), reads=["uf", "pp", kpy], writes=["uf"]))
                      its = [(st_, ks_) for st_ in range(c.NST) for ks_ in range(KS)]
                      per = max(1, -(-len(its) // KR))
                      nrec = 0
                      pend_b = None
                      for ii, (st_, ks_) in enumerate(its):
                          s5_iter(st_, ks_, ii % 2)
                          if pend_b is not None:
                              rec_b(pend_b)
                              pend_b = None
                          elif nrec < KR:
                              rec_a(nrec)
                              pend_b = nrec
                              nrec += 1
                      for fn_ in s5_late:
                          fn_()
                      del s5_late[:]
                      if pend_b is not None:
                          rec_b(pend_b)
                      while nrec < KR:
                          rec_a(nrec)
                          rec_b(nrec)
                          nrec += 1
                      for ks in range(KS):
                          op("act", lambda e, ks=ks: e.activation(out=gy[:, ks, :], in_=uf[:, ks, :], func=AF.Gelu),
                             reads=["uf"], writes=["gy"])
                      ck("M5")
                      for fo in range(KS):
                          for half, pb, pk in ((0, ps[2], "ps2"), (1, ps[3], "ps3")):
                              col = half * c.DS + fo * 128
                              for ks in range(KS):
                                  op("pe", lambda e, ks=ks, col=col, pb=pb: e.matmul(
                                      pb[:, 0:TT], lhsT=wglub[:, ks, col:col + 128], rhs=gy[:, ks, :],
                                      start=(ks == 0), stop=(ks == KS - 1)), reads=["wglub", "gy"], writes=[pk])
                          op("act", lambda e, fo=fo: e.activation(out=sgl[:], in_=ps[3][:, 0:TT], func=AF.Sigmoid,
                                                                  bias=P("bglu", KS + fo)),
                             reads=["ps3", "pp"], writes=["sgl"])
                          op("dve", lambda e, fo=fo: e.scalar_tensor_tensor(
                              out=yss[:], in0=ps[2][:, 0:TT], scalar=P("bglu", fo), in1=sgl[:], op0=ALU.add, op1=ALU.mult),
                             reads=["ps2", "pp", "sgl"], writes=["yss"])
                          op("act", lambda e, fo=fo: e.activation(out=ysq[:, KR + fo, :], in_=yss[:], func=AF.Square),
                             reads=["yss"], writes=["ysq"])
                          op("pool", lambda e, fo=fo: e.tensor_scalar(out=ybf[:, KR + fo, :], in0=yss[:],
                                                                      scalar1=P("gssm", fo), scalar2=0.0, op0=ALU.mult, op1=ALU.add),
                             reads=["yss", "pp"], writes=["ybf"])
                      ck("GLU")
                      chains = []
                      for j in range(JT):
                          jg = tt * JT + j
                          tsl = slice(j * 128, (j + 1) * 128)
                          rb = RB[j]
                          lg, exg, gm, etmp, ein, top8, sel1, sel2, oh1, oh2, Mt, Mb, valid, val, slf, x1b = (
                              rb[n] for n in ("lg", "exg", "gm", "etmp", "ein", "top8", "sel1", "sel2", "oh1", "oh2", "Mt",
                                              "Mb", "valid", "val", "slf", "x1b"))
                          zz, bst, mv, x1T = rb["zz"], rb["bst"], rb["mv"], rb["x1T"]
                          x1 = zz
                          rtj = rb["rt"]
                          if j % 2 == 0:
                              (pX, kX), (pY, kY), (pZ, kZ), (pR, kR) = (ps[4], "ps4"), (ps[0], "ps0"), (ps[1], "ps1"), (ps[6], "ps6")
                          else:
                              (pX, kX), (pY, kY), (pZ, kZ), (pR, kR) = (ps[5], "ps5"), (ps[2], "ps2"), (ps[3], "ps3"), (ps[2], "ps2")
                          ppos, kpos = pR, kR
                          chain = []
                          chains.append(chain)
                          km = lambda ks_, j=j: [(k + "#%d" % j) if k in CHAIN_KEYS else k for k in ks_]

                          def cop(eng, fn, reads=(), writes=(), chain=chain, km=km):
                              chain.append(("op", eng, _record(fn), None, km(reads), km(writes)))

                          def cdma(eng, fn, dsem, reads=(), writes=(), chain=chain, km=km):
                              chain.append(("dma", eng, _record(fn), dsem, km(reads), km(writes)))

                          for kc in range(KD):
                              half = 0 if kc < KR else 1
                              first = kc in (0, KR)
                              last = kc in (KR - 1, KD - 1)
                              cop("pe", lambda e, kc=kc, half=half, first=first, last=last: e.matmul(
                                  pX[:, half:half + 1], lhsT=ysq[:, kc, tsl], rhs=onesb[:, 0:1], start=first, stop=last),
                                 reads=["ysq", "onesb"], writes=[kX])
                          cop("act", lambda e: e.activation(out=rtj["ssr"][:], in_=pX[:, 0:1], func=AF.Sqrt,
                                                           scale=1.0 / c.DR, bias=1e-6), reads=[kX], writes=["rt_ssr"])
                          cop("act", lambda e: e.activation(out=rtj["sss"][:], in_=pX[:, 1:2], func=AF.Sqrt,
                                                           scale=1.0 / c.DS, bias=1e-6), reads=[kX], writes=["rt_sss"])
                          cop("dve", lambda e: e.reciprocal(out=rtj["rsr"][:], in_=rtj["ssr"][:]), reads=["rt_ssr"],
                             writes=["rt_rsr"])
                          cop("dve", lambda e: e.reciprocal(out=rtj["rss"][:], in_=rtj["sss"][:]), reads=["rt_sss"],
                             writes=["rt_rss"])
                          cop("act", lambda e, j=j: e.mul(out=xt[j][:], in_=xt[j][:], mul=c.ALPHA), reads=["xt%d" % j],
                             writes=["xt%d" % j])
                          for dh in range(c.NDH):
                              dsl = slice(dh * c.DW, (dh + 1) * c.DW)
                              for half, pb, pk in ((0, pY, kY), (1, pZ, kZ)):
                                  kcs = range(0, KR) if half == 0 else range(KR, KD)
                                  for kc in kcs:
                                      cop("pe", lambda e, kc=kc, pb=pb, kcs=kcs: e.matmul(
                                          pb[:, 0:c.DW], lhsT=ybf[:, kc, tsl], rhs=woutb[:, kc, dsl],
                                          start=(kc == kcs[0]), stop=(kc == kcs[-1])), reads=["ybf", "woutb"], writes=[pk])
                              cop("dve", lambda e, dsl=dsl, j=j: e.scalar_tensor_tensor(
                                  out=zz[:, dsl], in0=pY[:, 0:c.DW], scalar=rtj["rsr"][:], in1=xt[j][:, dsl],
                                  op0=ALU.mult, op1=ALU.add), reads=[kY, "rt_rsr", "xt%d" % j], writes=["zz"])
                              cop("dve", lambda e, dsl=dsl: e.scalar_tensor_tensor(
                                  out=zz[:, dsl], in0=pZ[:, 0:c.DW], scalar=rtj["rss"][:], in1=zz[:, dsl],
                                  op0=ALU.mult, op1=ALU.add), reads=[kZ, "rt_rss", "zz"], writes=["zz"])
                          layer_norm(c, cop, zz, x1, bst, mv, rtj, rows[:, 0:D], rows[:, D:2 * D], "zz", "zz", "rows")
                          cdma("sp", lambda e, jg=jg: e.dma_start(out=x1s[jg * 128:(jg + 1) * 128, :], in_=x1[:]), "x1st%d" % j,
                              reads=["zz"])
                          cop("act", lambda e, j=j: e.copy(out=x1b[:], in_=x1[:]), reads=["zz"], writes=["x1b#%d" % j])
                          ck("LN1")
                          for k0 in range(0, KD, 4):
                              kn = min(4, KD - k0)
                              for kk in range(kn):
                                  cop("pe", lambda e, kk=kk, k0=k0: e.transpose(
                                      out=pX[:, kk * 128:(kk + 1) * 128], in_=x1[:, (k0 + kk) * 128:(k0 + kk + 1) * 128],
                                      identity=ident[:]), reads=["zz", "ident"], writes=[kX])
                              cop("act", lambda e, k0=k0, kn=kn: e.copy(
                                  out=x1T[:, k0:k0 + kn, :], in_=pX[:, 0:kn * 128].rearrange("p (k t) -> p k t", k=kn)),
                                 reads=[kX], writes=["x1T"])
                          for kc in range(KD):
                              cop("pe", lambda e, kc=kc: e.matmul(pR[:, 0:c.NR], lhsT=x1T[:, kc, :], rhs=wrf[:, kc, :],
                                                                 start=(kc == 0), stop=(kc == KD - 1)),
                                 reads=["x1T", "wrf"], writes=[kR])
                          ck("RT0")
                          NG, EPG = c.NG, c.EPG

                          cop("dve", lambda e: e.tensor_tensor(out=lg[:], in0=pR[:, 0:c.NR], in1=rows[:, 2 * D:2 * D + c.NR],
                                                              op=ALU.add), reads=[kR, "rows"], writes=["lg"])
                          cop("dve", lambda e: e.tensor_reduce(out=rtj["gmax"][:], in_=lg[:, 0:NG], axis=AX.X, op=ALU.max),
                             reads=["lg"], writes=["rt_gmax"])
                          cop("dve", lambda e: e.tensor_scalar(out=rtj["ngmax"][:], in0=rtj["gmax"][:], scalar1=-1.0,
                                                              scalar2=None, op0=ALU.mult), reads=["rt_gmax"],
                             writes=["rt_ngmax"])
                          cop("act", lambda e: e.activation(out=exg[:], in_=lg[:, 0:NG], func=AF.Exp, bias=rtj["ngmax"][:],
                                                           accum_out=rtj["gsum"][:]), reads=["lg", "rt_ngmax"],
                             writes=["exg", "rt_gsum"])
                          cop("dve", lambda e: e.reciprocal(out=rtj["gtop"][:], in_=rtj["gsum"][:]), reads=["rt_gsum"],
                             writes=["rt_gtop"])
                          cop("dve", lambda e: e.tensor_scalar(out=gm[:], in0=lg[:, 0:NG], scalar1=rtj["gmax"][:],
                                                              scalar2=None, op0=ALU.is_equal), reads=["lg", "rt_gmax"],
                             writes=["gm"])
                          e3 = lambda ap: ap.rearrange("p (g e) -> p g e", g=NG)
                          gmb = gm[:].unsqueeze(2).to_broadcast([128, NG, EPG])
                          cop("dve", lambda e: e.tensor_tensor(out=e3(etmp[:]), in0=e3(lg[:, NG:NG + c.E]), in1=gmb,
                                                              op=ALU.mult), reads=["lg", "gm"], writes=["etmp"])
                          cop("dve", lambda e: e.tensor_reduce(out=ein[:], in_=etmp[:].rearrange("p (g e) -> p e g", g=NG),
                                                              axis=AX.X, op=ALU.add), reads=["etmp"], writes=["ein"])
                          cop("dve", lambda e: e.max(out=top8[:], in_=ein[:]), reads=["ein"], writes=["top8"])
                          cop("dve", lambda e: e.tensor_scalar(out=sel1[:], in0=ein[:], scalar1=top8[:, 0:1], scalar2=None,
                                                              op0=ALU.is_equal), reads=["ein", "top8"], writes=["sel1"])
                          cop("dve", lambda e: e.tensor_scalar(out=sel2[:], in0=ein[:], scalar1=top8[:, 1:2], scalar2=None,
                                                              op0=ALU.is_equal), reads=["ein", "top8"], writes=["sel2"])
                          cop("dve", lambda e: e.tensor_tensor(out=rtj["d12"][:], in0=top8[:, 0:1], in1=top8[:, 1:2],
                                                              op=ALU.subtract), reads=["top8"], writes=["rt_d12"])
                          cop("act", lambda e: e.activation(out=rtj["s1"][:], in_=rtj["d12"][:], func=AF.Sigmoid),
                             reads=["rt_d12"], writes=["rt_s1"])
                          cop("act", lambda e: e.activation(out=rtj["s2"][:], in_=rtj["d12"][:], func=AF.Sigmoid, scale=-1.0),
                             reads=["rt_d12"], writes=["rt_s2"])
                          for ohx, selx, kx in ((oh1, sel1, "1"), (oh2, sel2, "2")):
                              cop("dve", lambda e, ohx=ohx, selx=selx: e.tensor_tensor(
                                  out=e3(ohx[:]), in0=gmb, in1=selx[:].unsqueeze(1).to_broadcast([128, NG, EPG]),
                                  op=ALU.mult), reads=["gm", "sel" + kx], writes=["oh" + kx])
                          cop("dve", lambda e: e.tensor_tensor(out=Mt[:], in0=oh1[:], in1=oh2[:], op=ALU.add),
                             reads=["oh1", "oh2"], writes=["Mt"])
                          cop("dve", lambda e: e.tensor_copy(out=Mb[:], in_=Mt[:]), reads=["Mt"], writes=["Mb"])
                          cop("pe", lambda e: e.matmul(ppos[:, 64:64 + c.E], lhsT=ustr[:], rhs=Mb[:], start=True, stop=False),
                             reads=["ustr", "Mb"], writes=[kpos])
                          cop("pe", lambda e: e.matmul(ppos[:, 64:64 + c.E], lhsT=onesb[:], rhs=Mcumb[:], start=False,
                                                      stop=True), reads=["onesb", "Mcumb"], writes=[kpos])
                          cop("dve", lambda e: e.tensor_tensor(out=Mcum[:], in0=Mcum[:], in1=Mt[:], op=ALU.add),
                             reads=["Mcum", "Mt"], writes=["Mcum"])
                          cop("dve", lambda e: e.tensor_copy(out=Mcumb[:], in_=Mcum[:]), reads=["Mcum"], writes=["Mcumb"])
                          posp = ppos[:, 64:64 + c.E]
                          cop("dve", lambda e: e.tensor_scalar(out=valid[:], in0=posp, scalar1=float(C), scalar2=None,
                                                              op0=ALU.is_lt), reads=[kpos], writes=["valid"])
                          cop("dve", lambda e: e.tensor_tensor(out=val[:], in0=posp, in1=ebase[:, 0:E], op=ALU.add),
                             reads=[kpos, "ebase"], writes=["val"])
                          cop("dve", lambda e: e.scalar_tensor_tensor(out=val[:], in0=val[:], scalar=ebase[:, E:E + 1], in1=valid[:],
                                                                     op0=ALU.subtract, op1=ALU.mult),
                             reads=["val", "valid", "ebase"], writes=["val"])
                          cop("dve", lambda e: e.tensor_scalar(out=val[:], in0=val[:], scalar1=ebase[:, E:E + 1], scalar2=None,
                                                              op0=ALU.add), reads=["val", "ebase"], writes=["val"])
                          for ohx, kx, col in ((oh1, "1", 0), (oh2, "2", 1)):
                              cop("dve", lambda e, ohx=ohx: e.tensor_tensor(out=etmp[:], in0=ohx[:], in1=val[:], op=ALU.mult),
                                 reads=["oh" + kx, "val"], writes=["etmp"])
                              cop("dve", lambda e, col=col: e.tensor_reduce(out=slf[:, col:col + 1], in_=etmp[:], axis=AX.X,
                                                                           op=ALU.add), reads=["etmp"], writes=["slf"])
                              cop("dve", lambda e, ohx=ohx: e.tensor_tensor(out=etmp[:], in0=ohx[:], in1=valid[:],
                                                                           op=ALU.mult),
                                 reads=["oh" + kx, "valid"], writes=["etmp"])
                              cop("dve", lambda e, kx=kx: e.tensor_reduce(out=rtj["v" + kx][:], in_=etmp[:], axis=AX.X,
                                                                         op=ALU.add), reads=["etmp"], writes=["rt_v" + kx])
                              cop("dve", lambda e, kx=kx: e.tensor_tensor(out=rtj["g" + kx][:], in0=rtj["gtop"][:],
                                                                         in1=rtj["s" + kx][:], op=ALU.mult),
                                 reads=["rt_gtop", "rt_s" + kx], writes=["rt_g" + kx])
                              cop("dve", lambda e, kx=kx, col=col, jg=jg: e.tensor_tensor(
                                  out=GATES[:, jg, col:col + 1], in0=rtj["g" + kx][:], in1=rtj["v" + kx][:], op=ALU.mult),
                                 reads=["rt_g" + kx, "rt_v" + kx], writes=["GATES"])
                          cop("dve", lambda e, jg=jg: e.tensor_copy(out=SLOTS[:, jg, :], in_=slf[:]), reads=["slf"],
                             writes=["SLOTS"])
                          ck("RT1")
                          for col in range(2):
                              cdma("pool", lambda e, jg=jg, col=col: e.indirect_dma_start(
                                  out=xg, out_offset=bass.IndirectOffsetOnAxis(ap=SLOTS[:, jg, col:col + 1], axis=0),
                                  in_=x1b[:], in_offset=None), "scat",
                                  reads=["x1b", "SLOTS"])
                      OFFS = 6
                      nmax = max(len(ch) for ch in chains) + OFFS * (len(chains) - 1)
                      for s_ in range(nmax):
                          for ci, ch in enumerate(chains):
                              idx = s_ - ci * OFFS
                              if 0 <= idx < len(ch):
                                  kind_, eng_, rec_, dsem_, rd_, wr_ = ch[idx]
                                  if kind_ == "op":
                                      S.commit_op(eng_, rec_, rd_, wr_)
                                  else:
                                      S.commit_dma(eng_, rec_, dsem_, rd_, wr_)
              S.barrier()
              ck("E0")
              with ExitStack() as ee:
                  def se(name, shape, dt=F32):
                      return ee.enter_context(nc.sbuf_tensor("e%d_%s" % (l, name), list(shape), dt))

                  wgb = [se("wgb%d" % i, [128, KD, c.DE], BF16) for i in range(2)]
                  wub = [se("wub%d" % i, [128, KD, c.DE], BF16) for i in range(2)]
                  wdb = [se("wdb%d" % i, [128, KE, D], BF16) for i in range(2)]
                  xgt = [se("xgt%d" % i, [128, D], BF16) for i in range(2 * c.CB)]
                  xT = se("xT", [128, KD, C], BF16)
                  sg = [se("sg%d" % i, [128, C]) for i in range(2)]
                  hT = se("hT", [128, KE, C], BF16)
                  yo = [se("yo%d" % i, [128, D]) for i in range(2)]
                  stage_pool[0] = [se("estage%d" % i, [128, 2048]) for i in range(6)]
                  stage_pool[1] = "estage%d_" % l
                  stage_ctr[0] = 0

                  ecast = [0]
                  deferred = []

                  def load_expert(ei):
                      p = ei % 2
                      for cb in range(c.CB):
                          xi_ = p * c.CB + cb
                          r0 = ei * C + cb * 128
                          dma("sp", lambda e, xi_=xi_, r0=r0: e.dma_start(out=xgt[xi_][:], in_=xg[r0:r0 + 128, :]),
                              "xgt%d" % xi_, writes=["xgt%d" % xi_])
                      for (dst, src, K, F, key) in ((wgb[p], wg_d[l, ei], KD, c.DE, "wgb%d" % p),
                                                    (wub[p], wu_d[l, ei], KD, c.DE, "wub%d" % p),
                                                    (wdb[p], wd_d[l, ei], KE, D, "wdb%d" % p)):
                          sv = src.rearrange("(k p) f -> p k f", p=128)
                          kstep = max(1, 2048 // F)
                          for k0 in range(0, K, kstep):
                              k1 = min(K, k0 + kstep)
                              ceng = ("dve", "pool", "dve", "pool", "dve", "pool")[ecast[0] % 6]
                              ecast[0] += 1
                              load_cast(dst[:, k0:k1, :], sv[:, k0:k1, :], (k1 - k0) * F, key, shape3=(k1 - k0, F), eng=ceng,
                                        defer=(deferred if (ceng != "pool" and ei > 0) else None))

                  load_expert(0)
                  xcnt = 0
                  ycnt = 0
                  for ei in range(E):
                      p = ei % 2
                      if ei + 1 < E:
                          load_expert(ei + 1)
                      for cb in range(c.CB):
                          xi_ = p * c.CB + cb
                          for k0 in range(0, KD, 4):
                              kn = min(4, KD - k0)
                              for kk in range(kn):
                                  op("pe", lambda e, kk=kk, k0=k0, xi_=xi_: e.transpose(
                                      out=psT[:, kk * 128:(kk + 1) * 128],
                                      in_=xgt[xi_][:, (k0 + kk) * 128:(k0 + kk + 1) * 128], identity=identb[:]),
                                     reads=["xgt%d" % xi_, "identb"], writes=["psT"])
                              op("dve", lambda e, k0=k0, kn=kn, cb=cb: e.tensor_copy(
                                  out=xT[:, k0:k0 + kn, cb * 128:(cb + 1) * 128],
                                  in_=psT[:, 0:kn * 128].rearrange("p (k t) -> p k t", k=kn)),
                                 reads=["psT"], writes=["xT"])
                      for fc in range(KE):
                          pg, pu = ps[(fc % 2) * 2], ps[(fc % 2) * 2 + 1]
                          kg, ku = "ps%d" % ((fc % 2) * 2), "ps%d" % ((fc % 2) * 2 + 1)
                          for (pb, pk, wb, wk) in ((pg, kg, wgb[p], "wgb%d" % p), (pu, ku, wub[p], "wub%d" % p)):
                              for kc in range(KD):
                                  op("pe", lambda e, kc=kc, pb=pb, wb=wb, fc=fc: e.matmul(
                                      pb[:, 0:C], lhsT=wb[:, kc, fc * 128:(fc + 1) * 128], rhs=xT[:, kc, :],
                                      start=(kc == 0), stop=(kc == KD - 1)), reads=[wk, "xT"], writes=[pk])
                          si = fc % 2
                          op("act", lambda e, pg=pg, si=si: e.activation(out=sg[si][:], in_=pg[:, 0:C], func=AF.Silu),
                             reads=[kg], writes=["sg%d" % si])
                          op("dve", lambda e, pu=pu, si=si, fc=fc: e.tensor_tensor(out=hT[:, fc, :], in0=pu[:, 0:C],
                                                                                   in1=sg[si][:], op=ALU.mult),
                             reads=[ku, "sg%d" % si], writes=["hT"])
                      for fn_ in deferred:
                          fn_()
                      del deferred[:]
                      for cb in range(c.CB):
                          yi = ycnt % 2
                          ycnt += 1
                          for dh in range(c.NDH):
                              pb, pk = ps[4 + dh % 2], "ps%d" % (4 + dh % 2)
                              for fc in range(KE):
                                  op("pe", lambda e, fc=fc, pb=pb, cb=cb, dh=dh: e.matmul(
                                      pb[:, 0:c.DW], lhsT=hT[:, fc, cb * 128:(cb + 1) * 128],
                                      rhs=wdb[p][:, fc, dh * c.DW:(dh + 1) * c.DW], start=(fc == 0), stop=(fc == KE - 1)),
                                     reads=["hT", "wdb%d" % p], writes=[pk])
                              op("act", lambda e, pb=pb, yi=yi, dh=dh: e.copy(out=yo[yi][:, dh * c.DW:(dh + 1) * c.DW],
                                                                              in_=pb[:, 0:c.DW]),
                                 reads=[pk], writes=["yo%d" % yi])
                          r0 = ei * C + cb * 128
                          dma("sp", lambda e, yi=yi, r0=r0: e.dma_start(out=yg[r0:r0 + 128, :], in_=yo[yi][:]),
                              "yst%d" % yi, reads=["yo%d" % yi])
              S.barrier()
              ck("C0")
              with ExitStack() as ec:
                  def sc(name, shape, dt=F32):
                      return ec.enter_context(nc.sbuf_tensor("c%d_%s" % (l, name), list(shape), dt))

                  rows2 = sc("rows2", [128, 2 * D])
                  y1 = [sc("y1_%d" % i, [128, D]) for i in range(2)]
                  y2 = [sc("y2_%d" % i, [128, D]) for i in range(2)]
                  x1c = [sc("x1c%d" % i, [128, D]) for i in range(2)]
                  z2 = [sc("z2_%d" % i, [128, D]) for i in range(2)]
                  x2 = [sc("x2_%d" % i, [128, D]) for i in range(2)]
                  bst2 = sc("bst2", [128, 2 * c.NDH, 6])
                  mv2 = sc("mv2", [128, 2])
                  rt2 = {nm: sc("rt2_" + nm, [128, 1]) for nm in ("sd", "rstd")}
                  dma("sp", lambda e: e.dma_start(out=rows2[:], in_=rows_d[l][:, 2 * D:4 * D]), "lw2", writes=["rows2"])
                  for i in range(2):
                      op("dve", lambda e, i=i: e.memset(y1[i][:], 0.0), writes=["y1_%d" % i])
                      op("dve", lambda e, i=i: e.memset(y2[i][:], 0.0), writes=["y2_%d" % i])
                  for jg in range(c.NJ):
                      b = jg % 2
                      dma("pool", lambda e, jg=jg, b=b: e.indirect_dma_start(
                          out=y1[b][:], out_offset=None, in_=yg,
                          in_offset=bass.IndirectOffsetOnAxis(ap=SLOTS[:, jg, 0:1], axis=0)), "g1_%d" % b, reads=["SLOTS"], writes=["y1_%d" % b])
                      dma("pool", lambda e, jg=jg, b=b: e.indirect_dma_start(
                          out=y2[b][:], out_offset=None, in_=yg,
                          in_offset=bass.IndirectOffsetOnAxis(ap=SLOTS[:, jg, 1:2], axis=0)), "g2_%d" % b, reads=["SLOTS"], writes=["y2_%d" % b])
                      dma("sp", lambda e, jg=jg, b=b: e.dma_start(out=x1c[b][:], in_=x1s[jg * 128:(jg + 1) * 128, :]),
                          "x1c%d" % b, writes=["x1c%d" % b])
                      op("act", lambda e, b=b: e.mul(out=z2[b][:], in_=x1c[b][:], mul=c.ALPHA), reads=["x1c%d" % b],
                         writes=["z2_%d" % b])
                      op("dve", lambda e, b=b, jg=jg: e.scalar_tensor_tensor(
                          out=z2[b][:], in0=y1[b][:], scalar=GATES[:, jg, 0:1], in1=z2[b][:], op0=ALU.mult, op1=ALU.add),
                         reads=["y1_%d" % b, "GATES", "z2_%d" % b], writes=["z2_%d" % b])
                      op("dve", lambda e, b=b, jg=jg: e.scalar_tensor_tensor(
                          out=z2[b][:], in0=y2[b][:], scalar=GATES[:, jg, 1:2], in1=z2[b][:], op0=ALU.mult, op1=ALU.add),
                         reads=["y2_%d" % b, "GATES", "z2_%d" % b], writes=["z2_%d" % b])
                      layer_norm(c, op, z2[b], x2[b], bst2, mv2, rt2, rows2[:, 0:D], rows2[:, D:2 * D], "z2_%d" % b, "x2_%d" % b,
                                 "rows2", sfx="2")
                      dma("sp", lambda e, jg=jg, b=b: e.dma_start(out=xnext[jg * 128:(jg + 1) * 128, :], in_=x2[b][:]),
                          "x2st%d" % b, reads=["x2_%d" % b])
              S.barrier()
        except _Stop:
            pass
        S.barrier()
        S.emit()
    return nc


def layer_norm(c, op, src, dst, bst, mv, rt, g_row, b_row, ksrc, kdst, krows, sfx=""):
    D = c.D
    nch = 2 * c.NDH if D >= 512 else 1
    w = D // nch
    for i in range(nch):
        op("dve", lambda e, i=i: e.bn_stats(out=bst[:, i, :], in_=src[:, i * w:(i + 1) * w]), reads=[ksrc],
           writes=["bst" + sfx])
    op("dve", lambda e: e.bn_aggr(out=mv[:], in_=bst[:, 0:nch, :].rearrange("p a b -> p (a b)")), reads=["bst" + sfx],
       writes=["mv" + sfx])
    op("act", lambda e: e.activation(out=rt["sd"][:], in_=mv[:, 1:2], func=AF.Sqrt, bias=1e-5), reads=["mv" + sfx],
       writes=["rt_sd" + sfx])
    op("dve", lambda e: e.reciprocal(out=rt["rstd"][:], in_=rt["sd"][:]), reads=["rt_sd" + sfx], writes=["rt_rstd" + sfx])
    op("dve", lambda e: e.tensor_scalar(out=dst[:], in0=src[:], scalar1=mv[:, 0:1], scalar2=rt["rstd"][:],
                                        op0=ALU.subtract, op1=ALU.mult), reads=[ksrc, "mv" + sfx, "rt_rstd" + sfx],
       writes=[kdst])
    op("pool", lambda e: e.tensor_tensor(out=dst[:], in0=dst[:], in1=g_row, op=ALU.mult), reads=[kdst, krows],
       writes=[kdst])
    op("pool", lambda e: e.tensor_tensor(out=dst[:], in0=dst[:], in1=b_row, op=ALU.add), reads=[kdst, krows],
       writes=[kdst])


def prep_inputs(c, inp):
    L = c.L
    f = lambda a: np.ascontiguousarray(np.asarray(a, dtype=np.float32))
    out = {}
    out["w_in"] = f(inp["w_in"])
    out["w_glu"] = f(inp["w_glu"])
    out["w_out"] = f(inp["w_out"])
    KR, KS, GP = c.KR, c.KS, c.GP
    for src, dst in (("lru_wa", "lruA"), ("lru_wx", "lruX")):
        w = f(inp[src])
        blk = np.zeros((L, KR, 128, 128), np.float32)
        for kr in range(KR):
            for h2 in range(2):
                blk[:, kr, h2 * 64:(h2 + 1) * 64, h2 * 64:(h2 + 1) * 64] = w[:, kr * 2 + h2]
        out[dst] = blk
    for src, dst in (("ssm_b_re", "Bre"), ("ssm_b_im", "Bim")):
        b = f(inp[src])
        blk = np.zeros((L, 128, GP, 128), np.float32)
        for ks in range(KS):
            for q in range(4):
                for g2 in range(2):
                    g = ks * 8 + q * 2 + g2
                    p0 = q * 32 + g2 * 16
                    blk[:, p0:p0 + 16, ks * 4 + q, g2 * 64:(g2 + 1) * 64] = np.transpose(b[:, g], (0, 2, 1))
        out[dst] = blk
    for src, dst in (("ssm_c_re", "Cre"), ("ssm_c_im", "Cim")):
        cc = f(inp[src])
        blk = np.zeros((L, 128, GP, 32), np.float32)
        for gp in range(GP):
            for g2 in range(2):
                blk[:, g2 * 64:(g2 + 1) * 64, gp, g2 * 16:(g2 + 1) * 16] = np.transpose(cc[:, 2 * gp + g2], (0, 2, 1))
        out[dst] = blk
    pp = np.zeros((L, 128, c.NPP), np.float32)

    def put(name, arr):
        o, n = c.o[name]
        pp[:, :, o:o + n] = arr

    chan = lambda v, K: np.transpose(f(v).reshape(L, K, 128), (0, 2, 1))
    cw = f(inp["conv_w"])
    cwp = np.transpose(cw.reshape(L, 4, KR, 128), (0, 3, 2, 1)).reshape(L, 128, KR * 4)
    put("convw", cwp)
    put("convb", chan(inp["conv_b"], KR))
    put("ba", chan(inp["lru_ba"], KR))
    put("bx", chan(inp["lru_bx"], KR))
    put("lam", chan(inp["lru_lambda"], KR))
    put("grec", chan(inp["g_rec"], KR))
    put("bglu", chan(inp["b_glu"], 2 * KS))
    put("gssm", chan(inp["g_ssm"], KS))
    put("ssmd", chan(f(inp["ssm_d"]).reshape(L, -1), KS))
    gn = lambda v: np.transpose(f(v).reshape(L, GP, 2 * 64), (0, 2, 1))
    put("lre", gn(inp["ssm_lambda_re"]))
    put("lim", gn(inp["ssm_lambda_im"]))
    ldt = f(inp["ssm_log_dt"]).reshape(L, GP, 2, 1)
    put("ldt", np.transpose(np.broadcast_to(ldt, (L, GP, 2, 64)).reshape(L, GP, 128), (0, 2, 1)))
    out["pp"] = pp
    rows = np.concatenate([f(inp["ln1_g"]), f(inp["ln1_b"]), f(inp["ln2_g"]), f(inp["ln2_b"]),
                           f(inp["router_bg"]), f(inp["router_be"])], axis=1)
    out["rows"] = np.ascontiguousarray(np.broadcast_to(rows[:, None, :], (L, 128, c.NROW)))
    out["wr"] = np.ascontiguousarray(np.concatenate([f(inp["router_wg"]), f(inp["router_we"])], axis=2))
    out["wg"] = f(inp["exp_w_gate"])
    out["wu"] = f(inp["exp_w_up"])
    out["wd"] = f(inp["exp_w_down"])
    out["ident"] = np.eye(128, dtype=np.float32)
    out["ustrict"] = np.triu(np.ones((128, 128), np.float32), 1)
    eb = np.zeros((128, c.E + 1), np.float32)
    eb[:, :c.E] = (np.arange(c.E, dtype=np.float32) * c.C)[None, :]
    eb[:, c.E] = c.E * c.C + np.arange(128, dtype=np.float32)
    out["ebase"] = eb
    return out


_CACHE = {}


def kernel(**inputs):
    c = Cfg()
    x = np.asarray(inputs["x"], dtype=np.float32)
    B = x.shape[0]
    shared = prep_inputs(c, inputs)
    if "nc" not in _CACHE:
        _CACHE["nc"] = build(c)
    nc = _CACHE["nc"]
    in_maps = []
    for b in range(B):
        m = dict(shared)
        m["x"] = np.ascontiguousarray(x[b])
        in_maps.append(m)
    res = run_bass_kernel_spmd(nc, in_maps, core_ids=list(range(B)))
    return np.stack([np.asarray(res.results[b]["y"], dtype=np.float32) for b in range(B)], axis=0)
```
